# Optimizing a Trainium2 kernel written in Bass

```python
import jax, jax.numpy as jnp
from jax import lax
import numpy as np

D_MODEL = 1024
BATCH = 8
SEQ = 4096
DEPTH = 1

MEM_LEN = 256
EPS = 1e-6

LRU_WIDTH = D_MODEL
LRU_HEADS = 8
LRU_HEAD_DIM = LRU_WIDTH // LRU_HEADS
CONV_WIDTH = 4
LRU_C = 8.0

GLA_HEADS = 4
GLA_DV = D_MODEL // 8
GLA_DK = GLA_DV // 2
GLA_V_WIDTH = GLA_HEADS * GLA_DV
GLA_K_WIDTH = GLA_HEADS * GLA_DK
GLA_RANK = 16
GLA_TAU = 16.0
GLA_CHUNK = 64

XA_HEADS = 4
XA_HEAD_DIM = D_MODEL // 8
XA_WIDTH = XA_HEADS * XA_HEAD_DIM

MIX_WIDTH = LRU_WIDTH + GLA_V_WIDTH + XA_WIDTH
IN_SIZES = (LRU_WIDTH, GLA_K_WIDTH, GLA_K_WIDTH, GLA_V_WIDTH, GLA_RANK, XA_WIDTH, MIX_WIDTH)
IN_WIDTH = LRU_WIDTH + 2 * GLA_K_WIDTH + GLA_V_WIDTH + GLA_RANK + XA_WIDTH + MIX_WIDTH

kernel_name = "hybrid_rglru_gla_memxattn_parallel_heads"


def rms_norm(x, g):
    x32 = x.astype(jnp.float32)
    y = x32 * lax.rsqrt(jnp.mean(x32 * x32, axis=-1, keepdims=True) + EPS)
    return (y * g.astype(jnp.float32)).astype(x.dtype)


def causal_depthwise_conv(u, w, b):
    c = u.shape[-1]
    y = lax.conv_general_dilated(
        u, w[:, None, :].astype(u.dtype), window_strides=(1,),
        padding=[(CONV_WIDTH - 1, 0)], dimension_numbers=("NWC", "WIO", "NWC"),
        feature_group_count=c)
    return y + b.astype(u.dtype)


def rg_lru(u, w_a, b_a, w_i, b_i, lam):
    bsz, s, _ = u.shape
    uh = u.reshape(bsz, s, LRU_HEADS, LRU_HEAD_DIM)
    r = jax.nn.sigmoid((jnp.einsum("bshi,hij->bshj", uh, w_a) + b_a).astype(jnp.float32))
    i = jax.nn.sigmoid((jnp.einsum("bshi,hij->bshj", uh, w_i) + b_i).astype(jnp.float32))
    r = r.reshape(bsz, s, LRU_WIDTH)
    i = i.reshape(bsz, s, LRU_WIDTH)
    log_a = -LRU_C * r * jax.nn.softplus(-lam.astype(jnp.float32))
    a = jnp.exp(log_a)
    mult = jnp.sqrt(-jnp.expm1(2.0 * log_a))
    bx = mult * (i * u.astype(jnp.float32))

    def combine(left, right):
        a1, b1 = left
        a2, b2 = right
        return a1 * a2, a2 * b1 + b2

    _, h = lax.associative_scan(combine, (a, bx), axis=1)
    return h.astype(u.dtype)


def gla(q, k, v, g_lr, w_g2, b_g, norm_g):
    bsz, s, _ = q.shape
    n, c = s // GLA_CHUNK, GLA_CHUNK
    f32 = jnp.float32
    q = q.astype(f32).reshape(bsz, n, c, GLA_HEADS, GLA_DK) * (GLA_DK ** -0.5)
    k = k.astype(f32).reshape(bsz, n, c, GLA_HEADS, GLA_DK)
    v = v.astype(f32).reshape(bsz, n, c, GLA_HEADS, GLA_DV)
    logit = (g_lr @ w_g2 + b_g).astype(f32)
    log_alpha = jax.nn.log_sigmoid(logit) / GLA_TAU
    cum = jnp.cumsum(log_alpha.reshape(bsz, n, c, GLA_HEADS, GLA_DK), axis=2)
    cum_last = cum[:, :, -1:]
    q_dec = q * jnp.exp(cum)
    k_dec = k * jnp.exp(-cum)
    causal = jnp.tril(jnp.ones((c, c), dtype=bool))
    att = jnp.einsum("bnihk,bnjhk->bnhij", q_dec, k_dec)
    att = jnp.where(causal, att, 0.0)
    o_intra = jnp.einsum("bnhij,bnjhv->bnihv", att, v)
    kv = jnp.einsum("bnjhk,bnjhv->nbhkv", k * jnp.exp(cum_last - cum), v)
    decay = jnp.exp(cum_last[:, :, 0]).transpose(1, 0, 2, 3)

    def step(state, inp):
        d, kv_n = inp
        return d[..., None] * state + kv_n, state

    s0 = jnp.zeros((bsz, GLA_HEADS, GLA_DK, GLA_DV), f32)
    _, s_prev = lax.scan(step, s0, (decay, kv))
    o_inter = jnp.einsum("bnihk,nbhkv->bnihv", q_dec, s_prev)
    o = (o_intra + o_inter).reshape(bsz, s, GLA_HEADS, GLA_DV)
    o = o * lax.rsqrt(jnp.mean(o * o, axis=-1, keepdims=True) + EPS)
    o = o * norm_g.astype(f32).reshape(GLA_HEADS, GLA_DV)
    return o.reshape(bsz, s, GLA_V_WIDTH).astype(g_lr.dtype)


def memory_cross_attention(q, mem_n, w_mem_kv):
    bsz, s, _ = q.shape
    m = mem_n.shape[1]
    kvm = mem_n @ w_mem_kv
    km, vm = jnp.split(kvm, 2, axis=-1)
    qh = q.reshape(bsz, s, XA_HEADS, XA_HEAD_DIM)
    kh = km.reshape(bsz, m, XA_HEADS, XA_HEAD_DIM)
    vh = vm.reshape(bsz, m, XA_HEADS, XA_HEAD_DIM)
    scores = jnp.einsum("bshd,bmhd->bhsm", qh, kh).astype(jnp.float32) * (XA_HEAD_DIM ** -0.5)
    p = jax.nn.softmax(scores, axis=-1).astype(vh.dtype)
    o = jnp.einsum("bhsm,bmhd->bshd", p, vh)
    return o.reshape(bsz, s, XA_WIDTH)


def setup_inputs(seed: int = 0) -> dict:
    key = jax.random.key(seed)
    ks = jax.random.split(key, 20)
    f32 = jnp.float32
    nrm = lambda k, shape, scale: jax.random.normal(k, shape, f32) * scale
    x = jax.random.normal(ks[0], (BATCH, SEQ, D_MODEL), f32)
    mem = jax.random.normal(ks[1], (BATCH, MEM_LEN, D_MODEL), f32)
    norm_g = 1.0 + nrm(ks[2], (DEPTH, D_MODEL), 0.02)
    mem_norm_g = 1.0 + nrm(ks[3], (DEPTH, D_MODEL), 0.02)
    w_in = nrm(ks[4], (DEPTH, D_MODEL, IN_WIDTH), D_MODEL ** -0.5)
    conv_w = nrm(ks[5], (DEPTH, CONV_WIDTH, LRU_WIDTH), CONV_WIDTH ** -0.5)
    conv_b = nrm(ks[6], (DEPTH, LRU_WIDTH), 0.02)
    lru_w_a = nrm(ks[7], (DEPTH, LRU_HEADS, LRU_HEAD_DIM, LRU_HEAD_DIM), LRU_HEAD_DIM ** -0.5)
    lru_b_a = nrm(ks[8], (DEPTH, LRU_HEADS, LRU_HEAD_DIM), 0.02)
    lru_w_i = nrm(ks[9], (DEPTH, LRU_HEADS, LRU_HEAD_DIM, LRU_HEAD_DIM), LRU_HEAD_DIM ** -0.5)
    lru_b_i = nrm(ks[10], (DEPTH, LRU_HEADS, LRU_HEAD_DIM), 0.02)
    a_c = jax.random.uniform(ks[11], (DEPTH, LRU_WIDTH), f32, 0.9, 0.999)
    a0 = a_c ** (1.0 / LRU_C)
    lru_lambda = jnp.log(a0) - jnp.log1p(-a0)
    gla_w_g2 = nrm(ks[12], (DEPTH, GLA_RANK, GLA_K_WIDTH), GLA_RANK ** -0.5)
    gla_b_g = nrm(ks[13], (DEPTH, GLA_K_WIDTH), 0.02)
    gla_norm_g = 1.0 + nrm(ks[14], (DEPTH, GLA_V_WIDTH), 0.02)
    w_mem_kv = nrm(ks[15], (DEPTH, D_MODEL, 2 * XA_WIDTH), D_MODEL ** -0.5)
    w_out = nrm(ks[16], (DEPTH, MIX_WIDTH, D_MODEL), MIX_WIDTH ** -0.5)
    final_norm_g = 1.0 + nrm(ks[17], (D_MODEL,), 0.02)
    return {"x": x, "mem": mem, "norm_g": norm_g, "mem_norm_g": mem_norm_g, "w_in": w_in,
            "conv_w": conv_w, "conv_b": conv_b, "lru_w_a": lru_w_a, "lru_b_a": lru_b_a,
            "lru_w_i": lru_w_i, "lru_b_i": lru_b_i, "lru_lambda": lru_lambda,
            "gla_w_g2": gla_w_g2, "gla_b_g": gla_b_g, "gla_norm_g": gla_norm_g,
            "w_mem_kv": w_mem_kv, "w_out": w_out, "final_norm_g": final_norm_g}


def reference(x, mem, norm_g, mem_norm_g, w_in, conv_w, conv_b, lru_w_a, lru_b_a, lru_w_i,
              lru_b_i, lru_lambda, gla_w_g2, gla_b_g, gla_norm_g, w_mem_kv, w_out, final_norm_g):
    split_points = [int(v) for v in np.cumsum(IN_SIZES)[:-1]]
    for l in range(DEPTH):
        h = rms_norm(x, norm_g[l])
        proj = h @ w_in[l]
        u_lru, q_gla, k_gla, v_gla, g_lr, q_xa, gate = jnp.split(proj, split_points, axis=-1)
        u = causal_depthwise_conv(u_lru, conv_w[l], conv_b[l])
        y_lru = rg_lru(u, lru_w_a[l], lru_b_a[l], lru_w_i[l], lru_b_i[l], lru_lambda[l])
        y_gla = gla(q_gla, k_gla, v_gla, g_lr, gla_w_g2[l], gla_b_g[l], gla_norm_g[l])
        mem_n = rms_norm(mem, mem_norm_g[l])
        y_xa = memory_cross_attention(q_xa, mem_n, w_mem_kv[l])
        y = jnp.concatenate([y_lru, y_gla, y_xa], axis=-1) * jax.nn.silu(gate)
        x = x + y @ w_out[l]
    return rms_norm(x, final_norm_g)
```

```python
import contextlib
import numpy as np
import concourse.bass as bass
import concourse.mybir as mybir
from concourse.bass_utils import run_bass_kernel_spmd

F32 = mybir.dt.float32
BF16 = mybir.dt.bfloat16
AF = mybir.ActivationFunctionType
ALU = mybir.AluOpType

D = 1024
SEQ = 4096
NB = 8
MEM = 256
INW = 4624
MIXW = 2048
EPS = 1e-6
O_LRU, O_Q, O_K, O_V, O_GLR, O_XQ, O_GATE = 0, 1024, 1280, 1536, 2048, 2064, 2576
NPV = 96
PV_CONVW, PV_CONVB, PV_BA, PV_BI, PV_LAM, PV_NG, PV_MNG, PV_GNG = 0, 32, 40, 48, 56, 64, 72, 80
C_ID, C_L, C_U, C_SEL, C_ONE, NCC = 0, 128, 256, 384, 386, 514

ENGS = ("sp", "act", "pool", "pe", "dve")
SELF_SYNC = True


class Buf:
    __slots__ = ("name", "w", "r", "excl")

    def __init__(self, name, excl=False):
        self.name = name
        self.w = None
        self.r = {}
        self.excl = excl


class _Rec:
    def __init__(self):
        self.call = None

    def __getattr__(self, name):
        def f(*a, **k):
            self.call = (name, a, k)
            return self
        return f


def _bind(fn):
    r = _Rec()
    fn(r)
    name, a, k = r.call
    return lambda e: getattr(e, name)(*a, **k)


class Sched:
    def __init__(self):
        self.cmds = {e: [] for e in ENGS}
        self.cnt = {e: 0 for e in ENGS}
        self.seen = {e: {} for e in ENGS}
        self.dcnt = {}

    def _wait(self, eng, dep):
        key, val = dep
        if key == eng and (eng == "pe" or eng == "sp" or not SELF_SYNC):
            return
        if self.seen[eng].get(key, 0) >= val:
            return
        self.seen[eng][key] = val
        self.cmds[eng].append(("wait", key, val))

    def _deps(self, eng, reads, writes):
        for b in reads:
            if b.w is not None:
                self._wait(eng, b.w)
        for b in writes:
            if b.w is not None:
                self._wait(eng, b.w)
            for k, v in b.r.items():
                self._wait(eng, (k, v))

    def _mark(self, tok, reads, writes):
        for b in writes:
            b.w = tok
            b.r = {}
        for b in reads:
            b.r[tok[0]] = tok[1]

    def op(self, eng, fn, reads=(), writes=()):
        writes = list(writes) + [b for b in reads if b.excl]
        reads = [b for b in reads if not b.excl]
        self._deps(eng, reads, writes)
        self.cnt[eng] += 1
        tok = (eng, self.cnt[eng])
        self.cmds[eng].append(("op", _bind(fn), eng, 1))
        self._mark(tok, reads, writes)

    def dma(self, eng, fn, dkey, reads=(), writes=()):
        self._deps(eng, reads, writes)
        self.dcnt[dkey] = self.dcnt.get(dkey, 0) + 16
        tok = (dkey, self.dcnt[dkey])
        self.cmds[eng].append(("op", _bind(fn), dkey, 16))
        self._mark(tok, reads, writes)

    def barrier(self):
        toks = [(e, self.cnt[e]) for e in ENGS if self.cnt[e] > 0 and e != "sp"]
        toks += [(k, v) for k, v in self.dcnt.items()]
        for e in ENGS:
            for t in toks:
                if t[0] != e:
                    self._wait(e, t)

    def finish(self, eng="sp"):
        for k, v in self.dcnt.items():
            self._wait(eng, (k, v))


def build(S, T=256, dbg=False):
    NT = S // T
    NSUB = T // 128
    NCH = T // 64
    nc = bass.Bass("TRN2", target_bir_lowering=False)
    dt = lambda n, shp, kind="ExternalInput": nc.dram_tensor(n, shp, F32, kind=kind).ap()
    x_d = dt("x", [S, D])
    mem_d = dt("mem", [MEM, D])
    win_d = dt("w_in", [D, INW])
    wkv_d = dt("w_mem_kv", [D, D])
    wout_d = dt("w_out", [MIXW, D])
    wa_d = dt("wa", [128, 8 * 128])
    wi_d = dt("wi", [128, 8 * 128])
    pv_d = dt("pvec", [128, NPV])
    wg2b_d = dt("wg2b", [17, 256])
    fgb_d = dt("fgb", [128, D])
    cst_d = dt("cst", [128, NCC])
    out_d = dt("out", [S, D], kind="ExternalOutput")
    dbg_d = {}

    sch = Sched()
    es = contextlib.ExitStack()
    with es:
        def sb(name, shape, dtype=F32):
            return es.enter_context(nc.sbuf_tensor(name, shape, dtype))

        def ps(name, shape, dtype=F32):
            return es.enter_context(nc.psum_tensor(name, shape, dtype))

        W1 = sb("W1", [128, 8, INW], BF16); bW1 = Buf("W1")
        W2 = sb("W2", [128, 16, D], BF16); bW2 = Buf("W2")
        WA = sb("WA", [128, 8, 128], BF16); bWA = Buf("WA")
        WI = sb("WI", [128, 8, 128], BF16); bWI = Buf("WI")
        KT = sb("KT", [128, 4, MEM], BF16); bKT = Buf("KT")
        VM = sb("VM", [128, 2, 512], BF16); bVM = Buf("VM")
        FGB = sb("FGB", [128, D]); bFGB = Buf("FGB")
        CST = sb("CST", [128, NCC]); bCST = Buf("CST")
        IDB = sb("IDB", [128, 128], BF16); bIDB = Buf("IDB")
        ONB = sb("ONB", [128, 128], BF16); bONB = Buf("ONB")
        PV = sb("PV", [128, NPV]); bPV = Buf("PV")
        DP = sb("DP", [128, 40]); bDP = Buf("DP")
        WG = sb("WG", [17, 256]); bWG = Buf("WG")
        GLR = sb("GLR", [17, T]); bGLR = Buf("GLR")
        UH = sb("UH", [128, 8, 3]); bUH = Buf("UH")
        HST = sb("HST", [128, 8]); bHST = Buf("HST")
        S32 = sb("S32", [128, 2, 128]); bS32 = Buf("S32")
        SBF = sb("SBF", [128, 4, 128], BF16); bSBF = Buf("SBF")

        PB = [ps("pb%d" % i, [128, 512]) for i in range(7)]
        PTB = ps("ptb", [128, 1024], BF16)
        tok_ring = [(PB[0], Buf("ptok0", True)), (PB[1], Buf("ptok1", True))]
        tok_i = [0]

        def ptok():
            r = tok_ring[tok_i[0] % 2]
            tok_i[0] += 1
            return r

        mm_ring = []
        for bi in (2, 3, 4, 5):
            mm_ring.append((PB[bi], 0, Buf("pmm%d" % bi, True)))
        mm_i = [0]

        def pmm():
            t, off, b = mm_ring[mm_i[0] % len(mm_ring)]
            mm_i[0] += 1
            return t, off, b

        bATTE, bATTO = Buf("atte", True), Buf("atto", True)
        misc_ring = [(PB[4], 256, bATTE), (PB[5], 256, bATTO)]
        misc_i = [0]

        def pmisc():
            r = misc_ring[misc_i[0] % 2]
            misc_i[0] += 1
            return r

        bOPS = Buf("ops", True)
        bPTB = Buf("ptb", True)
        tp_ring = [(0, bPTB), (512, bPTB)]
        tp_i = [0]

        def ptp():
            r = tp_ring[tp_i[0] % 2]
            tp_i[0] += 1
            return r

        sems = {}

        def get_sem(key):
            if key not in sems:
                sems[key] = es.enter_context(nc.semaphore("s_" + key))
            return sems[key]

        for e in ENGS:
            get_sem(e)

        def ld(dst, src, b, key=None):
            sch.dma("sp", lambda e, d=dst, s=src: e.dma_start(out=d, in_=s), key or ("ld_" + b.name), writes=[b])

        ld(PV[:, :], pv_d[:, :], bPV)
        ld(CST[:, :], cst_d[:, :], bCST)
        ld(FGB[:, :], fgb_d[:, :], bFGB)
        ld(WG[:, :], wg2b_d[:, :], bWG)

        with contextlib.ExitStack() as ses:
            def ssb(name, shape, dtype=F32):
                return ses.enter_context(nc.sbuf_tensor(name, shape, dtype))

            STG = [ssb("stg%d" % i, [128, INW]) for i in range(3)]
            bSTG = [Buf("stg0"), Buf("stg1")]
            WKV = ssb("WKV", [128, 8, D], BF16); bWKV = Buf("WKV")
            MEMT = ssb("MEMT", [128, 8, MEM], BF16); bMEMT = Buf("MEMT")
            MX = ssb("MX", [128, D]); bMX = Buf("MX")
            MHB = ssb("MHB", [128, D], BF16); bMHB = Buf("MHB")
            SM = ssb("SM", [128, 8]); bSM = Buf("SM")
            TMP = ssb("TMPS", [128, 16]); bTMP = Buf("TMPS")

            sch.op("dve", lambda e: e.tensor_copy(out=IDB[:, :], in_=CST[:, C_ID:C_ID + 128]), reads=[bCST], writes=[bIDB])
            sch.op("dve", lambda e: e.tensor_copy(out=ONB[:, :], in_=CST[:, C_ONE:C_ONE + 128]), reads=[bCST], writes=[bONB])
            sch.op("dve", lambda e: e.tensor_scalar(out=DP[:, 0:8], in0=PV[:, PV_BA:PV_BA + 8], scalar1=0.5, scalar2=None, op0=ALU.mult), reads=[bPV], writes=[bDP])
            sch.op("dve", lambda e: e.tensor_scalar(out=DP[:, 8:16], in0=PV[:, PV_BI:PV_BI + 8], scalar1=0.5, scalar2=None, op0=ALU.mult), reads=[bPV], writes=[bDP])
            sch.op("act", lambda e: e.activation(out=TMP[:, 0:8], in_=PV[:, PV_LAM:PV_LAM + 8], func=AF.Exp, scale=-1.0), reads=[bPV], writes=[bTMP])
            sch.op("act", lambda e: e.activation(out=TMP[:, 8:16], in_=TMP[:, 0:8], func=AF.Ln, bias=1.0), reads=[bTMP], writes=[bTMP])
            sch.op("dve", lambda e: e.tensor_scalar(out=DP[:, 16:24], in0=TMP[:, 8:16], scalar1=-4.0, scalar2=None, op0=ALU.mult), reads=[bTMP], writes=[bDP])
            sch.op("dve", lambda e: e.tensor_scalar(out=DP[:, 24:32], in0=TMP[:, 8:16], scalar1=-8.0, scalar2=None, op0=ALU.mult), reads=[bTMP], writes=[bDP])
            sch.op("pool", lambda e: e.memset(UH[:, :, :], 0.0), writes=[bUH])
            sch.op("pool", lambda e: e.memset(HST[:, :], 0.0), writes=[bHST])
            sch.op("pool", lambda e: e.memset(S32[:, :, :], 0.0), writes=[bS32])
            sch.op("pool", lambda e: e.memset(SBF[:, :, :], 0.0), writes=[bSBF])
            sch.op("pool", lambda e: e.memset(GLR[:, :], 1.0), writes=[bGLR])

            HALF = INW // 2
            bSTGa = [Buf("stga%d" % i) for i in range(3)]
            bSTGb = [Buf("stgb%d" % i) for i in range(3)]

            def ldq(q, dst, src, b, key):
                sch.dma(q, lambda e, d=dst, s_=src: e.dma_start(out=d, in_=s_), key, writes=[b])

            ldq("sp", STG[0][:, 0:1024], wa_d[:, :], bSTGa[0], "stga0")
            ldq("act", STG[0][:, HALF:HALF + 1024], wi_d[:, :], bSTGb[0], "stgb0")
            sch.op("dve", lambda e: e.tensor_copy(out=WA[:, :, :], in_=STG[0][:, 0:1024].rearrange("p (c j) -> p c j", c=8)), reads=[bSTGa[0]], writes=[bWA])
            sch.op("act", lambda e: e.activation(out=WI[:, :, :], in_=STG[0][:, HALF:HALF + 1024].rearrange("p (c j) -> p c j", c=8), func=AF.Copy), reads=[bSTGb[0]], writes=[bWI])

            jobs = [("win", dc) for dc in range(8)] + [("wout", g) for g in range(4)] + [("wkv", g) for g in range(2)]

            def issue(ji):
                kind, i = jobs[ji]
                s_ = (ji + 1) % 3
                if kind == "win":
                    ldq("sp", STG[s_][:, 0:HALF], win_d[i * 128:(i + 1) * 128, 0:HALF], bSTGa[s_], "stga%d" % s_)
                    ldq("act", STG[s_][:, HALF:INW], win_d[i * 128:(i + 1) * 128, HALF:INW], bSTGb[s_], "stgb%d" % s_)
                else:
                    src = wout_d if kind == "wout" else wkv_d
                    ldq("sp", STG[s_][:, 0:2048].rearrange("p (c n) -> p c n", c=2), src[i * 512:i * 512 + 256, :].rearrange("(c p) n -> p c n", p=128), bSTGa[s_], "stga%d" % s_)
                    ldq("act", STG[s_][:, HALF:HALF + 2048].rearrange("p (c n) -> p c n", c=2), src[i * 512 + 256:(i + 1) * 512, :].rearrange("(c p) n -> p c n", p=128), bSTGb[s_], "stgb%d" % s_)

            def convert(ji):
                kind, i = jobs[ji]
                s_ = (ji + 1) % 3
                if kind == "win":
                    sch.op("dve", lambda e: e.tensor_scalar(out=W1[:, i, 0:HALF], in0=STG[s_][:, 0:HALF], scalar1=PV[:, PV_NG + i:PV_NG + i + 1], scalar2=None, op0=ALU.mult), reads=[bSTGa[s_], bPV], writes=[bW1])
                    sch.op("act", lambda e: e.activation(out=W1[:, i, HALF:INW], in_=STG[s_][:, HALF:INW], func=AF.Copy, scale=PV[:, PV_NG + i:PV_NG + i + 1]), reads=[bSTGb[s_], bPV], writes=[bW1])
                elif kind == "wout":
                    sch.op("dve", lambda e: e.tensor_copy(out=W2[:, 4 * i:4 * i + 2, :], in_=STG[s_][:, 0:2048].rearrange("p (c n) -> p c n", c=2)), reads=[bSTGa[s_]], writes=[bW2])
                    sch.op("act", lambda e: e.activation(out=W2[:, 4 * i + 2:4 * i + 4, :], in_=STG[s_][:, HALF:HALF + 2048].rearrange("p (c n) -> p c n", c=2), func=AF.Copy), reads=[bSTGb[s_]], writes=[bW2])
                else:
                    for c in range(2):
                        dc = 4 * i + c
                        sch.op("dve", lambda e, dc=dc, c=c: e.tensor_scalar(out=WKV[:, dc, :], in0=STG[s_][:, c * 1024:(c + 1) * 1024], scalar1=PV[:, PV_MNG + dc:PV_MNG + dc + 1], scalar2=None, op0=ALU.mult), reads=[bSTGa[s_], bPV], writes=[bWKV])
                    for c in range(2):
                        dc = 4 * i + 2 + c
                        sch.op("act", lambda e, dc=dc, c=c: e.activation(out=WKV[:, dc, :], in_=STG[s_][:, HALF + c * 1024:HALF + (c + 1) * 1024], func=AF.Copy, scale=PV[:, PV_MNG + dc:PV_MNG + dc + 1]), reads=[bSTGb[s_], bPV], writes=[bWKV])

            issue(0)
            issue(1)
            for ji in range(len(jobs)):
                if ji + 2 < len(jobs):
                    issue(ji + 2)
                convert(ji)
            for mc in range(2):
                ld(MX[:, :], mem_d[mc * 128:(mc + 1) * 128, :], bMX, key="mx")
                sch.op("act", lambda e, mc=mc: e.activation(out=MHB[:, :], in_=MX[:, :], func=AF.Square, accum_out=SM[:, mc:mc + 1]), reads=[bMX], writes=[bMHB, bSM])
                sch.op("act", lambda e, mc=mc: e.activation(out=SM[:, 2 + mc:3 + mc], in_=SM[:, mc:mc + 1], func=AF.Ln, scale=1.0 / D, bias=EPS), reads=[bSM], writes=[bSM])
                sch.op("act", lambda e, mc=mc: e.activation(out=SM[:, 4 + mc:5 + mc], in_=SM[:, 2 + mc:3 + mc], func=AF.Exp, scale=-0.5), reads=[bSM], writes=[bSM])
                sch.op("act", lambda e, mc=mc: e.activation(out=MHB[:, :], in_=MX[:, :], func=AF.Copy, scale=SM[:, 4 + mc:5 + mc]), reads=[bMX, bSM], writes=[bMHB])
                for hf in range(2):
                    off, bt = ptp()
                    for c in range(4):
                        dc = hf * 4 + c
                        sch.op("pe", lambda e, dc=dc, c=c, off=off: e.transpose(out=PTB[:, off + c * 128:off + (c + 1) * 128], in_=MHB[:, dc * 128:(dc + 1) * 128], identity=IDB[:, :]), reads=[bMHB, bIDB], writes=[bt])
                    sch.op("dve", lambda e, hf=hf, mc=mc, off=off: e.tensor_copy(out=MEMT[:, hf * 4:hf * 4 + 4, mc * 128:(mc + 1) * 128], in_=PTB[:, off:off + 512].rearrange("p (c t) -> p c t", c=4)), reads=[bt], writes=[bMEMT])
            for h in range(4):
                t, off, bp = pmm()
                for dc in range(8):
                    sch.op("pe", lambda e, h=h, dc=dc, t=t, off=off: e.matmul(out=t[:, off:off + MEM], lhsT=WKV[:, dc, h * 128:(h + 1) * 128], rhs=MEMT[:, dc, :], start=(dc == 0), stop=(dc == 7)), reads=[bWKV, bMEMT], writes=[bp])
                sch.op("act", lambda e, h=h, t=t, off=off: e.activation(out=KT[:, h, :], in_=t[:, off:off + MEM], func=AF.Copy), reads=[bp], writes=[bKT])
            for mc in range(2):
                t, bp = ptok()
                for dc in range(8):
                    sch.op("pe", lambda e, mc=mc, dc=dc, t=t: e.matmul(out=t[:, :], lhsT=MEMT[:, dc, mc * 128:(mc + 1) * 128], rhs=WKV[:, dc, 512:1024], start=(dc == 0), stop=(dc == 7)), reads=[bWKV, bMEMT], writes=[bp])
                sch.op("dve", lambda e, mc=mc, t=t: e.tensor_copy(out=VM[:, mc, :], in_=t[:, :]), reads=[bp], writes=[bVM])
            sch.barrier()

        NXL = 3
        XL = [sb("xl%d" % i, [128, D]) for i in range(NXL)]
        bXL = [Buf("xl%d" % i) for i in range(NXL)]
        xl_i = [0]

        def xl_alloc():
            k = xl_i[0] % NXL
            xl_i[0] += 1
            return k

        HB = [sb("hb%d" % i, [128, D], BF16) for i in range(NSUB)]
        bHB = [Buf("hb%d" % i) for i in range(NSUB)]
        SS = sb("SS", [128, 32]); bSS = [Buf("ss%d" % i) for i in range(8)]
        NEGH = sb("NEGH", [128, 1]); bNEGH = Buf("NEGH")
        HT = sb("HT", [128, 8, T], BF16); bHT = Buf("HT")
        SG = sb("SG", [128, 16, T], BF16); bSG = [Buf("sg%d" % i) for i in range(16)]
        YT = sb("YT", [128, 16, T], BF16); bYT = [Buf("yt%d" % i) for i in range(16)]
        LUr = [sb("lu%d" % i, [128, T + 3]) for i in range(2)]; bLU = [Buf("lu%d" % i) for i in range(2)]
        Lu = [sb("lu_%d" % i, [128, T]) for i in range(2)]; bLu = [Buf("lu_%d" % i) for i in range(2)]
        Lub = [sb("lub%d" % i, [128, T], BF16) for i in range(2)]; bLub = [Buf("lub%d" % i) for i in range(2)]
        Ltr = [sb("ltr%d" % i, [128, T]) for i in range(2)]; bLtr = [Buf("ltr%d" % i) for i in range(2)]
        Lti = [sb("lti%d" % i, [128, T]) for i in range(2)]; bLti = [Buf("lti%d" % i) for i in range(2)]
        Liu = [sb("liu%d" % i, [128, T], BF16) for i in range(4)]; bLiu = [Buf("liu%d" % i) for i in range(4)]
        La = [sb("la%d" % i, [128, T]) for i in range(4)]; bLa = [Buf("la%d" % i) for i in range(4)]
        Lm = [sb("lm%d" % i, [128, T]) for i in range(4)]; bLm = [Buf("lm%d" % i) for i in range(4)]
        Lh = [sb("lh%d" % i, [128, T]) for i in range(2)]; bLh = [Buf("lh%d" % i) for i in range(2)]
        LT = sb("LT", [128, T]); bLT = Buf("LT")
        QK0 = sb("qk0", [128, 512]); bQK0 = Buf("qk0")
        VT = [sb("vt%d" % i, [128, 512], BF16) for i in range(NSUB)]; bVT = [Buf("vt%d" % i) for i in range(NSUB)]
        GEj = [sb("GE%d" % i, [128, 256]) for i in range(NSUB)]; bGEj = [Buf("GE%d" % i) for i in range(NSUB)]
        GLAj = [sb("GLA%d" % i, [128, 256]) for i in range(NSUB)]; bGLAj = [Buf("GLA%d" % i) for i in range(NSUB)]
        GX = [sb("gx%d" % i, [128, 256]) for i in range(2)]; bGX = [Buf("gx%d" % i) for i in range(2)]
        QD = sb("QD", [128, 256], BF16); bQD = Buf("QD")
        KD = sb("KD", [128, 256], BF16); bKD = Buf("KD")
        KK = [sb("kk%d" % i, [128, 2, 256], BF16) for i in range(NSUB)]; bKK = [Buf("kk%d" % i) for i in range(NSUB)]
        QKT = sb("QKT", [128, 4, T], BF16); bQKT = Buf("QKT")
        DEC = sb("DEC", [128, 2, NCH]); bDEC = Buf("DEC")
        ATM = [sb("atm%d" % i, [128, 4, 128], BF16) for i in range(2)]; bATM = [Buf("atm%d" % i) for i in range(2)]
        GSQj = [sb("GSQ%d" % i, [128, 512], BF16) for i in range(NSUB)]; bGSQj = [Buf("GSQ%d" % i) for i in range(NSUB)]
        GRS = sb("GRS", [128, 512]); bGRS = Buf("GRS")
        QX = [sb("qx%d" % i, [128, 2, T], BF16) for i in range(1)]; bQX = [Buf("qx%d" % i) for i in range(1)]
        EX = [sb("ex%d" % i, [128, 2, T], BF16) for i in range(2)]; bEX = [Buf("ex%d" % i) for i in range(2)]
        RD = [sb("rd%d" % i, [128, T]) for i in range(2)]; bRD = [Buf("rd%d" % i) for i in range(2)]

        for i in range(NSUB):
            sch.op("pool", lambda e, i=i: e.memset(KK[i][:, :, :], 0.0), writes=[bKK[i]])
        sch.op("pool", lambda e: e.memset(NEGH[:, :], -0.5), writes=[bNEGH])

        pvc = lambda col: PV[:, col:col + 1]
        dpc = lambda col: DP[:, col:col + 1]
        ss_i = [0]

        def rms_rstd(src, bsrc, junk, bjunk, width):
            k = ss_i[0] % 8
            ss_i[0] += 1
            b = bSS[k]
            sch.op("act", lambda e: e.activation(out=junk, in_=src, func=AF.Square, accum_out=SS[:, 4 * k:4 * k + 1]), reads=[bsrc], writes=[bjunk, b])
            sch.op("pool", lambda e: e.tensor_scalar(out=SS[:, 4 * k + 1:4 * k + 2], in0=SS[:, 4 * k:4 * k + 1], scalar1=1.0 / width, scalar2=EPS, op0=ALU.mult, op1=ALU.add), reads=[b], writes=[b])
            sch.op("pool", lambda e: e.tensor_tensor(out=SS[:, 4 * k + 2:4 * k + 3], in0=SS[:, 4 * k + 1:4 * k + 2], in1=NEGH[:, 0:1], op=ALU.pow), reads=[b, bNEGH], writes=[b])
            return SS[:, 4 * k + 2:4 * k + 3], b

        def stageA_load(tt, j):
            t0 = tt * T + j * 128
            xi = xl_alloc()
            sch.dma("sp", lambda e, xi=xi, t0=t0: e.dma_start(out=XL[xi][:, :], in_=x_d[t0:t0 + 128, :]), "xl%d" % xi, writes=[bXL[xi]])
            rstd, brs = rms_rstd(XL[xi][:, :], bXL[xi], HB[j][:, :], bHB[j], D)
            sch.op("act", lambda e, xi=xi, j=j, rstd=rstd: e.activation(out=HB[j][:, :], in_=XL[xi][:, :], func=AF.Copy, scale=rstd), reads=[bXL[xi], brs], writes=[bHB[j]])

        def stageA_tr(j):
            for c in range(8):
                sch.op("pe", lambda e, c=c, j=j: e.transpose(out=PTB[:, c * 128:(c + 1) * 128], in_=HB[j][:, c * 128:(c + 1) * 128], identity=IDB[:, :]), reads=[bHB[j], bIDB], writes=[bPTB])
            sch.op("act", lambda e, j=j: e.activation(out=HT[:, :, j * 128:(j + 1) * 128], in_=PTB[:, :].rearrange("p (c t) -> p c t", c=8), func=AF.Copy), reads=[bPTB], writes=[bHT])

        def inproj_into(t, off, bp, col0, width=128):
            for dc in range(8):
                sch.op("pe", lambda e, dc=dc: e.matmul(out=t[0:width, off:off + T], lhsT=W1[:, dc, col0:col0 + width], rhs=HT[:, dc, :], start=(dc == 0), stop=(dc == 7)), reads=[bW1, bHT], writes=[bp])

        gate_done = set()
        inproj_left = [4]
        hb_cnt = [0]
        out_ok = [True]
        gate_i = [0]

        GATE_ORDER = (6, 0, 4, 7, 1, 5, 2, 3)
        NPRE = 4
        gate_pre = []

        def gate_mm(p):
            t, _, bp = pmm()
            inproj_into(t, 0, bp, O_GATE + 2 * p * 128)
            inproj_into(t, T, bp, O_GATE + (2 * p + 1) * 128)
            return t, bp

        def gate_preissue():
            for p in GATE_ORDER[:NPRE]:
                t, bp = gate_mm(p)
                gate_pre.append((p, t, bp))

        def gen_gate():
            pre = list(gate_pre)
            del gate_pre[:]
            todo = [(p, None, None) for p in GATE_ORDER[len(pre):]]
            first = True
            for (p, t, bp) in pre + todo:
                if t is None:
                    if pre and first:
                        first = False
                        yield
                    t, bp = gate_mm(p)
                    if p == GATE_ORDER[-1]:
                        inproj_left[0] -= 1
                sch.op("act", lambda e, t=t, p=p: e.activation(out=SG[:, 2 * p:2 * p + 2, :], in_=t[:, 0:2 * T].rearrange("p (c t) -> p c t", c=2), func=AF.Tanh, scale=0.5), reads=[bp], writes=bSG[2 * p:2 * p + 2])
                sch.op("dve", lambda e, t=t, p=p: e.scalar_tensor_tensor(out=SG[:, 2 * p:2 * p + 2, :], in0=SG[:, 2 * p:2 * p + 2, :], scalar=1.0, in1=t[:, 0:2 * T].rearrange("p (c t) -> p c t", c=2), op0=ALU.add, op1=ALU.mult), reads=[bp] + bSG[2 * p:2 * p + 2], writes=bSG[2 * p:2 * p + 2])
                gate_done.add(2 * p)
                gate_done.add(2 * p + 1)
                if t is not None and (p, t, bp) in pre:
                    continue
                yield
                if p not in GATE_ORDER[:3]:
                    yield

        def gla_tok_proj(j, col0, which):
            t, bp = ptok()
            for dc in range(8):
                sch.op("pe", lambda e, dc=dc: e.matmul(out=t[:, :], lhsT=HT[:, dc, j * 128:(j + 1) * 128], rhs=W1[:, dc, col0:col0 + 512], start=(dc == 0), stop=(dc == 7)), reads=[bW1, bHT], writes=[bp])
            if which == 0:
                sch.op("act", lambda e: e.activation(out=QK0[:, :], in_=t[:, :], func=AF.Copy), reads=[bp], writes=[bQK0])
            else:
                sch.op("act", lambda e: e.activation(out=VT[j][:, :], in_=t[:, :], func=AF.Copy), reads=[bp], writes=[bVT[j]])

        def gen_gla():
            t, _, bp = pmm()
            inproj_into(t, 0, bp, O_GLR, 16)
            sch.op("act", lambda e, t=t: e.activation(out=GLR[0:16, :], in_=t[0:16, 0:T], func=AF.Copy), reads=[bp], writes=[bGLR])
            yield
            for j in range(NSUB):
                gla_tok_proj(j, O_V, 1)
                yield
            for j in range(NSUB):
                t, _, bp = pmm()
                sch.op("pe", lambda e, t=t, j=j: e.matmul(out=t[:, 0:256], lhsT=GLR[:, j * 128:(j + 1) * 128], rhs=WG[:, :], start=True, stop=True), reads=[bGLR, bWG], writes=[bp])
                sch.op("act", lambda e, t=t, j=j: e.activation(out=GEj[j][:, :], in_=t[:, 0:256], func=AF.Exp, scale=-1.0), reads=[bp], writes=[bGEj[j]])
            for j in range(NSUB):
                sch.op("act", lambda e, j=j: e.activation(out=GLAj[j][:, :], in_=GEj[j][:, :], func=AF.Ln, bias=1.0), reads=[bGEj[j]], writes=[bGLAj[j]])
            yield
            for j in range(NSUB):
                gla_tok_proj(j, O_Q, 0)
                if j == NSUB - 1:
                    inproj_left[0] -= 1
                yield
                LA, bLA, EK, bEK = GLAj[j], bGLAj[j], GEj[j], bGEj[j]
                tc_, _, bpc = pmm()
                sch.op("pe", lambda e, t=tc_, LA=LA: e.matmul(out=t[:, 0:256], lhsT=CST[:, C_L:C_L + 128], rhs=LA[:, :], start=True, stop=True), reads=[bCST, bLA], writes=[bpc])
                sch.op("pe", lambda e, t=tc_, LA=LA: e.matmul(out=t[:, 256:512], lhsT=CST[:, C_U:C_U + 128], rhs=LA[:, :], start=True, stop=True), reads=[bCST, bLA], writes=[bpc])
                td_, _, bpd = pmm()
                for kc in range(2):
                    sch.op("pe", lambda e, t=td_, kc=kc, LA=LA: e.matmul(out=t[:, kc * 2:kc * 2 + 2], lhsT=LA[:, kc * 128:(kc + 1) * 128], rhs=CST[:, C_SEL:C_SEL + 2], start=True, stop=True), reads=[bCST, bLA], writes=[bpd])
                sch.op("act", lambda e, t=tc_: e.activation(out=GX[0][:, :], in_=t[:, 0:256], func=AF.Exp, scale=-1.0 / 16), reads=[bpc], writes=[bGX[0]])
                sch.op("act", lambda e, t=tc_: e.activation(out=GX[1][:, :], in_=t[:, 0:256], func=AF.Exp, scale=1.0 / 16), reads=[bpc], writes=[bGX[1]])
                sch.op("act", lambda e, t=tc_, EK=EK: e.activation(out=EK[:, :], in_=t[:, 256:512], func=AF.Exp, scale=-1.0 / 16), reads=[bpc], writes=[bEK])
                sch.op("act", lambda e, t=td_, j=j: e.activation(out=DEC[:, :, 2 * j:2 * j + 2], in_=t[:, 0:4].rearrange("p (k n) -> p k n", k=2), func=AF.Exp, scale=-1.0 / 16), reads=[bpd], writes=[bDEC])
                sch.op("dve", lambda e: e.scalar_tensor_tensor(out=QD[:, :], in0=QK0[:, 0:256], scalar=0.125, in1=GX[0][:, :], op0=ALU.mult, op1=ALU.mult), reads=[bQK0, bGX[0]], writes=[bQD])
                sch.op("dve", lambda e: e.tensor_tensor(out=KD[:, :], in0=QK0[:, 256:512], in1=GX[1][:, :], op=ALU.mult), reads=[bQK0, bGX[1]], writes=[bKD])
                for hf in range(2):
                    sch.op("dve", lambda e, j=j, hf=hf, EK=EK: e.tensor_tensor(out=KK[j][hf * 64:(hf + 1) * 64, hf, :], in0=QK0[hf * 64:(hf + 1) * 64, 256:512], in1=EK[hf * 64:(hf + 1) * 64, :], op=ALU.mult), reads=[bQK0, bEK], writes=[bKK[j]])
                for i4, (src, bsrc) in enumerate(((QD, bQD), (QD, bQD), (KD, bKD), (KD, bKD))):
                    kc = i4 % 2
                    sch.op("pe", lambda e, i4=i4, kc=kc, src=src: e.transpose(out=PTB[:, i4 * 128:(i4 + 1) * 128], in_=src[:, kc * 128:(kc + 1) * 128], identity=IDB[:, :]), reads=[bsrc, bIDB], writes=[bPTB])
                sch.op("act", lambda e, j=j: e.activation(out=QKT[:, :, j * 128:(j + 1) * 128], in_=PTB[:, 0:512].rearrange("p (c t) -> p c t", c=4), func=AF.Copy), reads=[bPTB], writes=[bQKT])
                yield
            for j in range(NSUB):
                tsl = slice(j * 128, (j + 1) * 128)
                am = j % 2
                te, _, bte = pmm()
                to, _, bto = pmm()
                for h in range(4):
                    kc, hh = h // 2, h % 2
                    tgt, bt = (te, bte) if hh == 0 else (to, bto)
                    sch.op("pe", lambda e, kc=kc, hh=hh, tgt=tgt: e.matmul(out=tgt[:, kc * 128:(kc + 1) * 128], lhsT=QKT[hh * 64:(hh + 1) * 64, 2 + kc, tsl], rhs=QKT[hh * 64:(hh + 1) * 64, kc, tsl], start=True, stop=True), reads=[bQKT], writes=[bt])
                for hh, (tgt, bt) in enumerate(((te, bte), (to, bto))):
                    for kc in range(2):
                        h = 2 * kc + hh
                        sch.op("dve", lambda e, tgt=tgt, kc=kc, h=h, am=am: e.tensor_tensor(out=ATM[am][:, h, :], in0=tgt[:, kc * 128:(kc + 1) * 128], in1=CST[:, C_L:C_L + 128], op=ALU.mult), reads=[bt, bCST], writes=[bATM[am]])
                yield
                for hf in range(2):
                    n = 2 * j + hf
                    csl = slice(j * 128 + hf * 64, j * 128 + hf * 64 + 64)
                    for h in range(4):
                        kc = h // 2
                        sch.op("pe", lambda e, h=h, hf=hf: e.matmul(out=PB[6][:, h * 128 + hf * 64:h * 128 + hf * 64 + 64], lhsT=VT[j][:, h * 128:(h + 1) * 128], rhs=ATM[am][:, h, hf * 64:(hf + 1) * 64], start=True, stop=False), reads=[bVT[j], bATM[am]], writes=[bOPS])
                        sch.op("pe", lambda e, h=h, hf=hf, kc=kc, csl=csl: e.matmul(out=PB[6][:, h * 128 + hf * 64:h * 128 + hf * 64 + 64], lhsT=SBF[:, h, :], rhs=QKT[:, kc, csl], start=False, stop=True), reads=[bSBF, bQKT], writes=[bOPS])
                    tk, _, bpk = pmm()
                    for kc in range(2):
                        sch.op("pe", lambda e, kc=kc, hf=hf, tk=tk: e.matmul(out=tk[:, kc * 256:(kc + 1) * 256], lhsT=KK[j][:, hf, kc * 128:(kc + 1) * 128], rhs=VT[j][:, kc * 256:(kc + 1) * 256], start=True, stop=True), reads=[bKK[j], bVT[j]], writes=[bpk])
                    for kc in range(2):
                        for hh in range(2):
                            psl = slice(hh * 64, (hh + 1) * 64)
                            sch.op("dve", lambda e, kc=kc, hh=hh, psl=psl, n=n, tk=tk: e.scalar_tensor_tensor(out=S32[psl, kc, :], in0=S32[psl, kc, :], scalar=DEC[psl, kc, n:n + 1], in1=tk[psl, kc * 256 + hh * 128:kc * 256 + (hh + 1) * 128], op0=ALU.mult, op1=ALU.add), reads=[bS32, bDEC, bpk], writes=[bS32])
                    for hh in range(2):
                        psl = slice(hh * 64, (hh + 1) * 64)
                        sch.op("dve", lambda e, hh=hh, psl=psl: e.tensor_copy(out=SBF[psl, hh:4:2, :], in_=S32[psl, 0:2, :]), reads=[bS32], writes=[bSBF])
                    yield
                    yield
                sch.op("act", lambda e, j=j: e.activation(out=GSQj[j][:, :], in_=PB[6][:, :], func=AF.Square), reads=[bOPS], writes=[bGSQj[j]])
                while not (out_ok[0] and all((8 + h) in gate_done for h in range(4))):
                    yield
                for h in range(4):
                    fc = 8 + h
                    sch.op("dve", lambda e, h=h, fc=fc: e.scalar_tensor_tensor(out=YT[:, fc, tsl], in0=PB[6][:, h * 128:(h + 1) * 128], scalar=pvc(PV_GNG + h), in1=SG[:, fc, tsl], op0=ALU.mult, op1=ALU.mult), reads=[bOPS, bPV, bSG[fc]], writes=[bYT[fc]])
                yield
            hb_ = [ptok() for _ in range(NSUB)]
            for j in range(NSUB):
                t, bp = hb_[j]
                sch.op("pe", lambda e, t=t, j=j: e.matmul(out=t[:, :], lhsT=ONB[:, :], rhs=GSQj[j][:, :], start=True, stop=True), reads=[bONB, bGSQj[j]], writes=[bp])
            for j in range(NSUB):
                t, bp = hb_[j]
                sch.op("act", lambda e, t=t: e.activation(out=t[:, :], in_=t[:, :], func=AF.Ln, scale=1.0 / 128, bias=EPS), reads=[bp], writes=[bp])
            for j in range(NSUB):
                t, bp = hb_[j]
                tsl = slice(j * 128, (j + 1) * 128)
                sch.op("act", lambda e, t=t: e.activation(out=GRS[:, :], in_=t[:, :], func=AF.Exp, scale=-0.5), reads=[bp], writes=[bGRS])
                sch.op("dve", lambda e, tsl=tsl: e.scalar_tensor_tensor(out=YT[:, 8:12, tsl], in0=YT[:, 8:12, tsl], scalar=0.5, in1=GRS[:, :].rearrange("p (h t) -> p h t", h=4), op0=ALU.mult, op1=ALU.mult), reads=[bGRS] + bYT[8:12], writes=bYT[8:12])
            yield

        def gen_xa():
            for hp in range(2):
                t, _, bp = pmm()
                for q in range(2):
                    inproj_into(t, q * T, bp, O_XQ + (2 * hp + q) * 128)
                if hp == 1:
                    inproj_left[0] -= 1
                sch.op("act", lambda e, t=t: e.activation(out=QX[0][:, :, :], in_=t[:, 0:2 * T].rearrange("p (c t) -> p c t", c=2), func=AF.Copy), reads=[bp], writes=[bQX[0]])
                yield
                for q in range(2):
                    h = 2 * hp + q
                    k = h % 2
                    fc = 12 + h
                    ts_, _, bps = pmm()
                    for mc in range(2):
                        sch.op("pe", lambda e, mc=mc, h=h, q=q, ts_=ts_: e.matmul(out=ts_[:, mc * T:(mc + 1) * T], lhsT=KT[:, h, mc * 128:(mc + 1) * 128], rhs=QX[0][:, q, :], start=True, stop=True), reads=[bKT, bQX[0]], writes=[bps])
                    sch.op("act", lambda e, k=k, ts_=ts_: e.activation(out=EX[k][:, :, :], in_=ts_[:, 0:2 * T].rearrange("p (c t) -> p c t", c=2), func=AF.Exp, scale=128.0 ** -0.5), reads=[bps], writes=[bEX[k]])
                    yield
                    to_, _, bpo = pmm()
                    for mc in range(2):
                        sch.op("pe", lambda e, mc=mc, h=h, k=k, to_=to_: e.matmul(out=to_[:, 0:T], lhsT=VM[:, mc, h * 128:(h + 1) * 128], rhs=EX[k][:, mc, :], start=(mc == 0), stop=(mc == 1)), reads=[bVM, bEX[k]], writes=[bpo])
                    for mc in range(2):
                        sch.op("pe", lambda e, mc=mc, k=k, to_=to_: e.matmul(out=to_[:, T:2 * T], lhsT=ONB[:, :], rhs=EX[k][:, mc, :], start=(mc == 0), stop=(mc == 1)), reads=[bONB, bEX[k]], writes=[bpo])
                    sch.op("dve", lambda e, k=k, to_=to_: e.reciprocal(out=RD[k][:, :], in_=to_[:, T:2 * T]), reads=[bpo], writes=[bRD[k]])
                    sch.op("dve", lambda e, k=k, to_=to_: e.scalar_tensor_tensor(out=RD[k][:, :], in0=to_[:, 0:T], scalar=0.5, in1=RD[k][:, :], op0=ALU.mult, op1=ALU.mult), reads=[bpo, bRD[k]], writes=[bRD[k]])
                    while not (out_ok[0] and fc in gate_done):
                        yield
                    sch.op("pool", lambda e, k=k, fc=fc: e.tensor_tensor(out=YT[:, fc, :], in0=RD[k][:, :], in1=SG[:, fc, :], op=ALU.mult), reads=[bRD[k], bSG[fc]], writes=[bYT[fc]])
                    yield

        lru_i = [0]

        def gen_lru():
            def stepA(c):
                s = c % 2
                t, _, bp = pmm()
                inproj_into(t, 0, bp, O_LRU + c * 128)
                if c == 7:
                    inproj_left[0] -= 1
                sch.op("dve", lambda e: e.tensor_copy(out=LUr[s][:, 3:3 + T], in_=t[:, 0:T]), reads=[bp], writes=[bLU[s]])
                sch.op("pool", lambda e: e.tensor_copy(out=LUr[s][:, 0:3], in_=UH[:, c, :]), reads=[bUH], writes=[bLU[s]])
                cw = lambda k_: pvc(PV_CONVW + c * 4 + k_)
                sch.op("pool", lambda e: e.tensor_scalar(out=Lu[s][:, :], in0=LUr[s][:, 3:3 + T], scalar1=cw(3), scalar2=pvc(PV_CONVB + c), op0=ALU.mult, op1=ALU.add), reads=[bLU[s], bPV], writes=[bLu[s]])
                sch.op("pool", lambda e: e.tensor_scalar(out=LT[:, :], in0=LUr[s][:, 2:2 + T], scalar1=cw(2), scalar2=0.0, op0=ALU.mult, op1=ALU.add), reads=[bLU[s], bPV], writes=[bLT])
                sch.op("pool", lambda e: e.tensor_tensor(out=Lu[s][:, :], in0=Lu[s][:, :], in1=LT[:, :], op=ALU.add), reads=[bLT, bLu[s]], writes=[bLu[s]])
                for k_ in (1, 0):
                    sch.op("pool", lambda e: e.tensor_scalar(out=LT[:, :], in0=LUr[s][:, k_:k_ + T], scalar1=cw(k_), scalar2=0.0, op0=ALU.mult, op1=ALU.add), reads=[bLU[s], bPV], writes=[bLT])
                    sch.op("pool", lambda e: e.tensor_tensor(out=Lu[s][:, :], in0=Lu[s][:, :], in1=LT[:, :], op=ALU.add), reads=[bLT, bLu[s]], writes=[bLu[s]])
                sch.op("pool", lambda e: e.tensor_copy(out=UH[:, c, :], in_=LUr[s][:, T:T + 3]), reads=[bLU[s]], writes=[bUH])
                sch.op("pool", lambda e: e.tensor_copy(out=Lub[s][:, :], in_=Lu[s][:, :]), reads=[bLu[s]], writes=[bLub[s]])

            def stepB(c):
                s = c % 2
                r4 = c % 4
                tz, _, bpz = pmm()
                sch.op("pe", lambda e: e.matmul(out=tz[:, 0:T], lhsT=WA[:, c, :], rhs=Lub[s][:, :], start=True, stop=True), reads=[bWA, bLub[s]], writes=[bpz])
                sch.op("pe", lambda e: e.matmul(out=tz[:, T:2 * T], lhsT=WI[:, c, :], rhs=Lub[s][:, :], start=True, stop=True), reads=[bWI, bLub[s]], writes=[bpz])
                sch.op("act", lambda e: e.activation(out=Ltr[s][:, :], in_=tz[:, 0:T], func=AF.Tanh, scale=0.5, bias=dpc(c)), reads=[bpz, bDP], writes=[bLtr[s]])
                sch.op("act", lambda e: e.activation(out=Lti[s][:, :], in_=tz[:, T:2 * T], func=AF.Tanh, scale=0.5, bias=dpc(8 + c)), reads=[bpz, bDP], writes=[bLti[s]])
                sch.op("act", lambda e: e.activation(out=La[r4][:, :], in_=Ltr[s][:, :], func=AF.Exp, scale=dpc(16 + c), bias=dpc(16 + c)), reads=[bLtr[s], bDP], writes=[bLa[r4]])
                sch.op("act", lambda e: e.activation(out=Lm[r4][:, :], in_=Ltr[s][:, :], func=AF.Exp, scale=dpc(24 + c), bias=dpc(24 + c)), reads=[bLtr[s], bDP], writes=[bLm[r4]])
                sch.op("dve", lambda e: e.scalar_tensor_tensor(out=Liu[r4][:, :], in0=Lti[s][:, :], scalar=1.0, in1=Lu[s][:, :], op0=ALU.add, op1=ALU.mult), reads=[bLti[s], bLu[s]], writes=[bLiu[r4]])

            def stepL():
                for r4 in range(4):
                    sch.op("act", lambda e: e.activation(out=Lm[r4][:, :], in_=Lm[r4][:, :], func=AF.Sqrt, scale=-0.0625, bias=0.0625), reads=[bLm[r4]], writes=[bLm[r4]])

            def stepC(c):
                r4 = c % 4
                s2 = c % 2
                sch.op("dve", lambda e: e.tensor_tensor(out=Lm[r4][:, :], in0=Lm[r4][:, :], in1=Liu[r4][:, :], op=ALU.mult), reads=[bLm[r4], bLiu[r4]], writes=[bLm[r4]])
                sch.op("dve", lambda e: e.tensor_tensor_scan(out=Lh[s2][:, :], data0=La[r4][:, :], data1=Lm[r4][:, :], initial=HST[:, c:c + 1], op0=ALU.mult, op1=ALU.add), reads=[bLa[r4], bLm[r4], bHST], writes=[bLh[s2]])
                sch.op("pool", lambda e: e.tensor_copy(out=HST[:, c:c + 1], in_=Lh[s2][:, T - 1:T]), reads=[bLh[s2]], writes=[bHST])
                while not (out_ok[0] and c in gate_done):
                    yield
                sch.op("pool", lambda e: e.tensor_tensor(out=YT[:, c, :], in0=Lh[s2][:, :], in1=SG[:, c, :], op=ALU.mult), reads=[bLh[s2], bSG[c]], writes=[bYT[c]])

            order = ["A0", "A1", "B0", "A2", "B1", "A3", "B2", "A4", "B3", "A5", "L", "C0", "B4", "C1", "A6", "B5", "C2", "A7", "B6", "C3", "B7", "L", "C4", "C5", "C6", "C7"]
            for st in order:
                if st[0] == "A":
                    stepA(int(st[1]))
                elif st[0] == "B":
                    stepB(int(st[1]))
                elif st[0] == "L":
                    stepL()
                else:
                    yield from stepC(int(st[1]))
                yield

        def gen_prefetch(tt):
            for _ in range(6):
                yield
            while not out_ok[0]:
                yield
            if tt + 1 < NT:
                for j in range(NSUB):
                    stageA_load(tt + 1, j)
                    hb_cnt[0] += 1
                    yield
            for j in range(NSUB):
                t0 = tt * T + j * 128
                xi = xl_alloc()
                sch.dma("sp", lambda e, xi=xi, t0=t0: e.dma_start(out=XL[xi][:, :], in_=x_d[t0:t0 + 128, :]), "xl%d" % xi, writes=[bXL[xi]])
                xo_slots.append(xi)
                yield

        def gen_tr(tt):
            if tt + 1 < NT:
                while inproj_left[0] > 0 or hb_cnt[0] < NSUB:
                    yield
                for j in range(NSUB):
                    stageA_tr(j)
                yield

        xo_slots = []

        def out_stage(tt):
            for j in range(NSUB):
                t0 = tt * T + j * 128
                xi = xo_slots.pop(0)
                for nh in range(2):
                    t, bp = ptok()
                    for fc in range(16):
                        sch.op("pe", lambda e, t=t, fc=fc, j=j, nh=nh: e.matmul(out=t[:, :], lhsT=YT[:, fc, j * 128:(j + 1) * 128], rhs=W2[:, fc, nh * 512:(nh + 1) * 512], start=(fc == 0), stop=(fc == 15)), reads=[bYT[fc], bW2], writes=[bp])
                    sch.op("dve", lambda e, t=t, xi=xi, nh=nh: e.tensor_tensor(out=XL[xi][:, nh * 512:(nh + 1) * 512], in0=XL[xi][:, nh * 512:(nh + 1) * 512], in1=t[:, :], op=ALU.add), reads=[bXL[xi], bp], writes=[bXL[xi]])
                rstd, brs = rms_rstd(XL[xi][:, :], bXL[xi], HB[j][:, :], bHB[j], D)
                sch.op("dve", lambda e, xi=xi, rstd=rstd: e.scalar_tensor_tensor(out=XL[xi][:, :], in0=XL[xi][:, :], scalar=rstd, in1=FGB[:, :], op0=ALU.mult, op1=ALU.mult), reads=[bXL[xi], brs, bFGB], writes=[bXL[xi]])
                sch.dma("sp", lambda e, xi=xi, t0=t0: e.dma_start(out=out_d[t0:t0 + 128, :], in_=XL[xi][:, :]), "xo%d" % xi, reads=[bXL[xi]])


        def interleave(gens, rounds=None):
            while gens and (rounds is None or rounds > 0):
                if rounds is not None:
                    rounds -= 1
                for gw in list(gens):
                    g, w = gw
                    for _ in range(w):
                        try:
                            next(g)
                        except StopIteration:
                            gens.remove(gw)
                            break

        for j in range(NSUB):
            stageA_load(0, j)
        EARLY_ROUNDS = 2
        for j in range(NSUB):
            stageA_tr(j)
        def mk_streams(tt_):
            inproj_left[0] = 4
            hb_cnt[0] = 0
            return [[gen_gate(), 2], [gen_gla(), 2], [gen_lru(), 2], [gen_xa(), 1], [gen_prefetch(tt_), 1], [gen_tr(tt_), 1]]
        streams = mk_streams(0)
        for tt in range(NT):
            interleave(streams)
            gate_done.clear()
            if tt + 1 < NT:
                out_ok[0] = False
                streams = mk_streams(tt + 1)
                interleave(streams, rounds=EARLY_ROUNDS)
            out_stage(tt)
            out_ok[0] = True

        sch.finish("sp")

        for key in list(sch.dcnt.keys()):
            get_sem(key)

        def replay(name, eng):
            for cmd in sch.cmds[name]:
                if cmd[0] == "wait":
                    eng.wait_ge(sems[cmd[1]], cmd[2])
                else:
                    _, fn, key, inc = cmd
                    fn(eng).then_inc(sems[key], inc)

        with nc.Block() as block:
            @block.sync
            def _(e):
                replay("sp", e)

            @block.scalar
            def _(e):
                replay("act", e)

            @block.gpsimd
            def _(e):
                replay("pool", e)

            @block.tensor
            def _(e):
                replay("pe", e)

            @block.vector
            def _(e):
                replay("dve", e)
    return nc


def _consts():
    c = np.zeros((128, NCC), np.float32)
    idx = np.arange(128)
    c[:, C_ID:C_ID + 128] = np.eye(128, dtype=np.float32)
    same = (idx[:, None] // 64) == (idx[None, :] // 64)
    c[:, C_L:C_L + 128] = (same & (idx[:, None] <= idx[None, :])).astype(np.float32)
    c[:, C_U:C_U + 128] = (same & (idx[:, None] > idx[None, :])).astype(np.float32)
    c[:, C_SEL] = (idx < 64).astype(np.float32)
    c[:, C_SEL + 1] = (idx >= 64).astype(np.float32)
    c[:, C_ONE:C_ONE + 128] = 1.0
    return c


def host_layout(inp, b, S):
    f = lambda a: np.ascontiguousarray(np.asarray(a, dtype=np.float32))
    chunked = lambda v: f(v).reshape(-1, 128).T
    pv = np.zeros((128, NPV), np.float32)
    cw = f(inp["conv_w"][0])
    for c in range(8):
        for k in range(4):
            pv[:, PV_CONVW + c * 4 + k] = cw[k, c * 128:(c + 1) * 128]
    pv[:, PV_CONVB:PV_CONVB + 8] = chunked(inp["conv_b"][0])
    pv[:, PV_BA:PV_BA + 8] = f(inp["lru_b_a"][0]).T
    pv[:, PV_BI:PV_BI + 8] = f(inp["lru_b_i"][0]).T
    pv[:, PV_LAM:PV_LAM + 8] = chunked(inp["lru_lambda"][0])
    pv[:, PV_NG:PV_NG + 8] = chunked(inp["norm_g"][0])
    pv[:, PV_MNG:PV_MNG + 8] = chunked(inp["mem_norm_g"][0])
    pv[:, PV_GNG:PV_GNG + 4] = chunked(inp["gla_norm_g"][0])
    wg2b = np.concatenate([f(inp["gla_w_g2"][0]), f(inp["gla_b_g"][0])[None, :]], axis=0)
    return {
        "x": f(inp["x"][b, :S]),
        "mem": f(inp["mem"][b]),
        "w_in": f(inp["w_in"][0]),
        "w_mem_kv": f(inp["w_mem_kv"][0]),
        "w_out": f(inp["w_out"][0]),
        "wa": f(np.transpose(f(inp["lru_w_a"][0]), (1, 0, 2)).reshape(128, 1024)),
        "wi": f(np.transpose(f(inp["lru_w_i"][0]), (1, 0, 2)).reshape(128, 1024)),
        "pvec": pv,
        "wg2b": f(wg2b),
        "fgb": f(np.broadcast_to(f(inp["final_norm_g"])[None, :], (128, D))),
        "cst": _consts(),
    }


def kernel(**inputs):
    nc = build(SEQ)
    in_maps = [host_layout(inputs, b, SEQ) for b in range(NB)]
    res = run_bass_kernel_spmd(nc, in_maps, core_ids=list(range(NB)))
    return np.stack([np.asarray(r["out"], dtype=np.float32) for r in res.results], axis=0)
```

```python
import contextlib
import numpy as np
import concourse.bass as bass
import concourse.mybir as mybir
from concourse.bass_utils import run_bass_kernel_spmd

F32 = mybir.dt.float32
BF16 = mybir.dt.bfloat16
AF = mybir.ActivationFunctionType
ALU = mybir.AluOpType

D = 1024
SEQ = 4096
NB = 8
MEM = 256
INW = 4624
MIXW = 2048
EPS = 1e-6
O_LRU, O_Q, O_K, O_V, O_GLR, O_XQ, O_GATE = 0, 1024, 1280, 1536, 2048, 2064, 2576
NPV = 96
PV_CONVW, PV_CONVB, PV_BA, PV_BI, PV_LAM, PV_NG, PV_MNG, PV_GNG = 0, 32, 40, 48, 56, 64, 72, 80
C_ID, C_L, C_U, C_SEL, C_ONE, NCC = 0, 128, 256, 384, 386, 514

ENGS = ("sp", "act", "pool", "pe", "dve")
SELF_SYNC = True


class Buf:
    __slots__ = ("name", "w", "r", "excl")

    def __init__(self, name, excl=False):
        self.name = name
        self.w = None
        self.r = {}
        self.excl = excl


class _Rec:
    def __init__(self):
        self.call = None

    def __getattr__(self, name):
        def f(*a, **k):
            self.call = (name, a, k)
            return self
        return f


def _bind(fn):
    r = _Rec()
    fn(r)
    name, a, k = r.call
    return lambda e: getattr(e, name)(*a, **k)


class Sched:
    def __init__(self):
        self.cmds = {e: [] for e in ENGS}
        self.cnt = {e: 0 for e in ENGS}
        self.seen = {e: {} for e in ENGS}
        self.dcnt = {}

    def _wait(self, eng, dep):
        key, val = dep
        if key == eng and (eng == "pe" or eng == "sp" or not SELF_SYNC):
            return
        if self.seen[eng].get(key, 0) >= val:
            return
        self.seen[eng][key] = val
        self.cmds[eng].append(("wait", key, val))

    def _deps(self, eng, reads, writes):
        for b in reads:
            if b.w is not None:
                self._wait(eng, b.w)
        for b in writes:
            if b.w is not None:
                self._wait(eng, b.w)
            for k, v in b.r.items():
                self._wait(eng, (k, v))

    def _mark(self, tok, reads, writes):
        for b in writes:
            b.w = tok
            b.r = {}
        for b in reads:
            b.r[tok[0]] = tok[1]

    def op(self, eng, fn, reads=(), writes=()):
        writes = list(writes) + [b for b in reads if b.excl]
        reads = [b for b in reads if not b.excl]
        self._deps(eng, reads, writes)
        self.cnt[eng] += 1
        tok = (eng, self.cnt[eng])
        self.cmds[eng].append(("op", _bind(fn), eng, 1))
        self._mark(tok, reads, writes)

    def dma(self, eng, fn, dkey, reads=(), writes=()):
        self._deps(eng, reads, writes)
        self.dcnt[dkey] = self.dcnt.get(dkey, 0) + 16
        tok = (dkey, self.dcnt[dkey])
        self.cmds[eng].append(("op", _bind(fn), dkey, 16))
        self._mark(tok, reads, writes)

    def barrier(self):
        toks = [(e, self.cnt[e]) for e in ENGS if self.cnt[e] > 0 and e != "sp"]
        toks += [(k, v) for k, v in self.dcnt.items()]
        for e in ENGS:
            for t in toks:
                if t[0] != e:
                    self._wait(e, t)

    def finish(self, eng="sp"):
        for k, v in self.dcnt.items():
            self._wait(eng, (k, v))


def build(S, T=256, dbg=False):
    NT = S // T
    NSUB = T // 128
    NCH = T // 64
    nc = bass.Bass("TRN2", target_bir_lowering=False)
    dt = lambda n, shp, kind="ExternalInput": nc.dram_tensor(n, shp, F32, kind=kind).ap()
    x_d = dt("x", [S, D])
    mem_d = dt("mem", [MEM, D])
    win_d = dt("w_in", [D, INW])
    wkv_d = dt("w_mem_kv", [D, D])
    wout_d = dt("w_out", [MIXW, D])
    wa_d = dt("wa", [128, 8 * 128])
    wi_d = dt("wi", [128, 8 * 128])
    pv_d = dt("pvec", [128, NPV])
    wg2b_d = dt("wg2b", [17, 256])
    fgb_d = dt("fgb", [128, D])
    cst_d = dt("cst", [128, NCC])
    out_d = dt("out", [S, D], kind="ExternalOutput")
    dbg_d = {}

    sch = Sched()
    es = contextlib.ExitStack()
    with es:
        def sb(name, shape, dtype=F32):
            return es.enter_context(nc.sbuf_tensor(name, shape, dtype))

        def ps(name, shape, dtype=F32):
            return es.enter_context(nc.psum_tensor(name, shape, dtype))

        W1 = sb("W1", [128, 8, INW], BF16); bW1 = Buf("W1")
        W2 = sb("W2", [128, 16, D], BF16); bW2 = Buf("W2")
        WA = sb("WA", [128, 8, 128], BF16); bWA = Buf("WA")
        WI = sb("WI", [128, 8, 128], BF16); bWI = Buf("WI")
        KT = sb("KT", [128, 4, MEM], BF16); bKT = Buf("KT")
        VM = sb("VM", [128, 2, 512], BF16); bVM = Buf("VM")
        FGB = sb("FGB", [128, D]); bFGB = Buf("FGB")
        CST = sb("CST", [128, NCC]); bCST = Buf("CST")
        IDB = sb("IDB", [128, 128], BF16); bIDB = Buf("IDB")
        ONB = sb("ONB", [128, 128], BF16); bONB = Buf("ONB")
        PV = sb("PV", [128, NPV]); bPV = Buf("PV")
        DP = sb("DP", [128, 40]); bDP = Buf("DP")
        WG = sb("WG", [17, 256]); bWG = Buf("WG")
        GLR = sb("GLR", [17, T]); bGLR = Buf("GLR")
        UH = sb("UH", [128, 8, 3]); bUH = Buf("UH")
        HST = sb("HST", [128, 8]); bHST = Buf("HST")
        S32 = sb("S32", [128, 2, 128]); bS32 = Buf("S32")
        SBF = sb("SBF", [128, 4, 128], BF16); bSBF = Buf("SBF")

        PB = [ps("pb%d" % i, [128, 512]) for i in range(7)]
        PTB = ps("ptb", [128, 1024], BF16)
        tok_ring = [(PB[0], Buf("ptok0", True)), (PB[1], Buf("ptok1", True))]
        tok_i = [0]

        def ptok():
            r = tok_ring[tok_i[0] % 2]
            tok_i[0] += 1
            return r

        mm_ring = []
        for bi in (2, 3, 4, 5):
            mm_ring.append((PB[bi], 0, Buf("pmm%d" % bi, True)))
        mm_i = [0]

        def pmm():
            t, off, b = mm_ring[mm_i[0] % len(mm_ring)]
            mm_i[0] += 1
            return t, off, b

        bATTE, bATTO = Buf("atte", True), Buf("atto", True)
        misc_ring = [(PB[4], 256, bATTE), (PB[5], 256, bATTO)]
        misc_i = [0]

        def pmisc():
            r = misc_ring[misc_i[0] % 2]
            misc_i[0] += 1
            return r

        bOPS = Buf("ops", True)
        bPTB = Buf("ptb", True)
        tp_ring = [(0, bPTB), (512, bPTB)]
        tp_i = [0]

        def ptp():
            r = tp_ring[tp_i[0] % 2]
            tp_i[0] += 1
            return r

        sems = {}

        def get_sem(key):
            if key not in sems:
                sems[key] = es.enter_context(nc.semaphore("s_" + key))
            return sems[key]

        for e in ENGS:
            get_sem(e)

        def ld(dst, src, b, key=None):
            sch.dma("sp", lambda e, d=dst, s=src: e.dma_start(out=d, in_=s), key or ("ld_" + b.name), writes=[b])

        ld(PV[:, :], pv_d[:, :], bPV)
        ld(CST[:, :], cst_d[:, :], bCST)
        ld(FGB[:, :], fgb_d[:, :], bFGB)
        ld(WG[:, :], wg2b_d[:, :], bWG)

        with contextlib.ExitStack() as ses:
            def ssb(name, shape, dtype=F32):
                return ses.enter_context(nc.sbuf_tensor(name, shape, dtype))

            STG = [ssb("stg%d" % i, [128, INW]) for i in range(3)]
            bSTG = [Buf("stg0"), Buf("stg1")]
            WKV = ssb("WKV", [128, 8, D], BF16); bWKV = Buf("WKV")
            MEMT = ssb("MEMT", [128, 8, MEM], BF16); bMEMT = Buf("MEMT")
            MX = ssb("MX", [128, D]); bMX = Buf("MX")
            MHB = ssb("MHB", [128, D], BF16); bMHB = Buf("MHB")
            SM = ssb("SM", [128, 8]); bSM = Buf("SM")
            TMP = ssb("TMPS", [128, 16]); bTMP = Buf("TMPS")

            sch.op("dve", lambda e: e.tensor_copy(out=IDB[:, :], in_=CST[:, C_ID:C_ID + 128]), reads=[bCST], writes=[bIDB])
            sch.op("dve", lambda e: e.tensor_copy(out=ONB[:, :], in_=CST[:, C_ONE:C_ONE + 128]), reads=[bCST], writes=[bONB])
            sch.op("dve", lambda e: e.tensor_scalar(out=DP[:, 0:8], in0=PV[:, PV_BA:PV_BA + 8], scalar1=0.5, scalar2=None, op0=ALU.mult), reads=[bPV], writes=[bDP])
            sch.op("dve", lambda e: e.tensor_scalar(out=DP[:, 8:16], in0=PV[:, PV_BI:PV_BI + 8], scalar1=0.5, scalar2=None, op0=ALU.mult), reads=[bPV], writes=[bDP])
            sch.op("act", lambda e: e.activation(out=TMP[:, 0:8], in_=PV[:, PV_LAM:PV_LAM + 8], func=AF.Exp, scale=-1.0), reads=[bPV], writes=[bTMP])
            sch.op("act", lambda e: e.activation(out=TMP[:, 8:16], in_=TMP[:, 0:8], func=AF.Ln, bias=1.0), reads=[bTMP], writes=[bTMP])
            sch.op("dve", lambda e: e.tensor_scalar(out=DP[:, 16:24], in0=TMP[:, 8:16], scalar1=-4.0, scalar2=None, op0=ALU.mult), reads=[bTMP], writes=[bDP])
            sch.op("dve", lambda e: e.tensor_scalar(out=DP[:, 24:32], in0=TMP[:, 8:16], scalar1=-8.0, scalar2=None, op0=ALU.mult), reads=[bTMP], writes=[bDP])
            sch.op("pool", lambda e: e.memset(UH[:, :, :], 0.0), writes=[bUH])
            sch.op("pool", lambda e: e.memset(HST[:, :], 0.0), writes=[bHST])
            sch.op("pool", lambda e: e.memset(S32[:, :, :], 0.0), writes=[bS32])
            sch.op("pool", lambda e: e.memset(SBF[:, :, :], 0.0), writes=[bSBF])
            sch.op("pool", lambda e: e.memset(GLR[:, :], 1.0), writes=[bGLR])

            HALF = INW // 2
            bSTGa = [Buf("stga%d" % i) for i in range(3)]
            bSTGb = [Buf("stgb%d" % i) for i in range(3)]

            def ldq(q, dst, src, b, key):
                sch.dma(q, lambda e, d=dst, s_=src: e.dma_start(out=d, in_=s_), key, writes=[b])

            ldq("sp", STG[0][:, 0:1024], wa_d[:, :], bSTGa[0], "stga0")
            ldq("act", STG[0][:, HALF:HALF + 1024], wi_d[:, :], bSTGb[0], "stgb0")
            sch.op("dve", lambda e: e.tensor_copy(out=WA[:, :, :], in_=STG[0][:, 0:1024].rearrange("p (c j) -> p c j", c=8)), reads=[bSTGa[0]], writes=[bWA])
            sch.op("act", lambda e: e.activation(out=WI[:, :, :], in_=STG[0][:, HALF:HALF + 1024].rearrange("p (c j) -> p c j", c=8), func=AF.Copy), reads=[bSTGb[0]], writes=[bWI])

            jobs = [("win", dc) for dc in range(8)] + [("wout", g) for g in range(4)] + [("wkv", g) for g in range(2)]

            def issue(ji):
                kind, i = jobs[ji]
                s_ = (ji + 1) % 3
                if kind == "win":
                    ldq("sp", STG[s_][:, 0:HALF], win_d[i * 128:(i + 1) * 128, 0:HALF], bSTGa[s_], "stga%d" % s_)
                    ldq("act", STG[s_][:, HALF:INW], win_d[i * 128:(i + 1) * 128, HALF:INW], bSTGb[s_], "stgb%d" % s_)
                else:
                    src = wout_d if kind == "wout" else wkv_d
                    ldq("sp", STG[s_][:, 0:2048].rearrange("p (c n) -> p c n", c=2), src[i * 512:i * 512 + 256, :].rearrange("(c p) n -> p c n", p=128), bSTGa[s_], "stga%d" % s_)
                    ldq("act", STG[s_][:, HALF:HALF + 2048].rearrange("p (c n) -> p c n", c=2), src[i * 512 + 256:(i + 1) * 512, :].rearrange("(c p) n -> p c n", p=128), bSTGb[s_], "stgb%d" % s_)

            def convert(ji):
                kind, i = jobs[ji]
                s_ = (ji + 1) % 3
                if kind == "win":
                    sch.op("dve", lambda e: e.tensor_scalar(out=W1[:, i, 0:HALF], in0=STG[s_][:, 0:HALF], scalar1=PV[:, PV_NG + i:PV_NG + i + 1], scalar2=None, op0=ALU.mult), reads=[bSTGa[s_], bPV], writes=[bW1])
                    sch.op("act", lambda e: e.activation(out=W1[:, i, HALF:INW], in_=STG[s_][:, HALF:INW], func=AF.Copy, scale=PV[:, PV_NG + i:PV_NG + i + 1]), reads=[bSTGb[s_], bPV], writes=[bW1])
                elif kind == "wout":
                    sch.op("dve", lambda e: e.tensor_copy(out=W2[:, 4 * i:4 * i + 2, :], in_=STG[s_][:, 0:2048].rearrange("p (c n) -> p c n", c=2)), reads=[bSTGa[s_]], writes=[bW2])
                    sch.op("act", lambda e: e.activation(out=W2[:, 4 * i + 2:4 * i + 4, :], in_=STG[s_][:, HALF:HALF + 2048].rearrange("p (c n) -> p c n", c=2), func=AF.Copy), reads=[bSTGb[s_]], writes=[bW2])
                else:
                    for c in range(2):
                        dc = 4 * i + c
                        sch.op("dve", lambda e, dc=dc, c=c: e.tensor_scalar(out=WKV[:, dc, :], in0=STG[s_][:, c * 1024:(c + 1) * 1024], scalar1=PV[:, PV_MNG + dc:PV_MNG + dc + 1], scalar2=None, op0=ALU.mult), reads=[bSTGa[s_], bPV], writes=[bWKV])
                    for c in range(2):
                        dc = 4 * i + 2 + c
                        sch.op("act", lambda e, dc=dc, c=c: e.activation(out=WKV[:, dc, :], in_=STG[s_][:, HALF + c * 1024:HALF + (c + 1) * 1024], func=AF.Copy, scale=PV[:, PV_MNG + dc:PV_MNG + dc + 1]), reads=[bSTGb[s_], bPV], writes=[bWKV])

            issue(0)
            issue(1)
            mem_dram_rows = lambda mc: mem_d[mc * 128:(mc + 1) * 128, :]
            for mc in range(2):
                sch.dma("pool", lambda e, mc=mc: e.dma_start(out=MX[:, :], in_=mem_dram_rows(mc)), "mx", writes=[bMX])
                sch.op("act", lambda e, mc=mc: e.activation(out=MHB[:, :], in_=MX[:, :], func=AF.Square, accum_out=SM[:, mc:mc + 1]), reads=[bMX], writes=[bMHB, bSM])
                sch.op("act", lambda e, mc=mc: e.activation(out=SM[:, 2 + mc:3 + mc], in_=SM[:, mc:mc + 1], func=AF.Ln, scale=1.0 / D, bias=EPS), reads=[bSM], writes=[bSM])
                sch.op("act", lambda e, mc=mc: e.activation(out=SM[:, 4 + mc:5 + mc], in_=SM[:, 2 + mc:3 + mc], func=AF.Exp, scale=-0.5), reads=[bSM], writes=[bSM])
                sch.op("act", lambda e, mc=mc: e.activation(out=MHB[:, :], in_=MX[:, :], func=AF.Copy, scale=SM[:, 4 + mc:5 + mc]), reads=[bMX, bSM], writes=[bMHB])
                for hf in range(2):
                    off, bt = ptp()
                    for c in range(4):
                        dc = hf * 4 + c
                        sch.op("pe", lambda e, dc=dc, c=c, off=off: e.transpose(out=PTB[:, off + c * 128:off + (c + 1) * 128], in_=MHB[:, dc * 128:(dc + 1) * 128], identity=IDB[:, :]), reads=[bMHB, bIDB], writes=[bt])
                    sch.op("dve", lambda e, hf=hf, mc=mc, off=off: e.tensor_copy(out=MEMT[:, hf * 4:hf * 4 + 4, mc * 128:(mc + 1) * 128], in_=PTB[:, off:off + 512].rearrange("p (c t) -> p c t", c=4)), reads=[bt], writes=[bMEMT])
            for ji in range(len(jobs)):
                if ji + 2 < len(jobs):
                    issue(ji + 2)
                convert(ji)
            for h in range(4):
                t, off, bp = pmm()
                for dc in range(8):
                    sch.op("pe", lambda e, h=h, dc=dc, t=t, off=off: e.matmul(out=t[:, off:off + MEM], lhsT=WKV[:, dc, h * 128:(h + 1) * 128], rhs=MEMT[:, dc, :], start=(dc == 0), stop=(dc == 7)), reads=[bWKV, bMEMT], writes=[bp])
                sch.op("act", lambda e, h=h, t=t, off=off: e.activation(out=KT[:, h, :], in_=t[:, off:off + MEM], func=AF.Copy), reads=[bp], writes=[bKT])
            for mc in range(2):
                t, bp = ptok()
                for dc in range(8):
                    sch.op("pe", lambda e, mc=mc, dc=dc, t=t: e.matmul(out=t[:, :], lhsT=MEMT[:, dc, mc * 128:(mc + 1) * 128], rhs=WKV[:, dc, 512:1024], start=(dc == 0), stop=(dc == 7)), reads=[bWKV, bMEMT], writes=[bp])
                sch.op("dve", lambda e, mc=mc, t=t: e.tensor_copy(out=VM[:, mc, :], in_=t[:, :]), reads=[bp], writes=[bVM])
            sch.barrier()

        NXL = 3
        XL = [sb("xl%d" % i, [128, D]) for i in range(NXL)]
        bXL = [Buf("xl%d" % i) for i in range(NXL)]
        xl_i = [0]

        def xl_alloc():
            k = xl_i[0] % NXL
            xl_i[0] += 1
            return k

        HB = [sb("hb%d" % i, [128, D], BF16) for i in range(NSUB)]
        bHB = [Buf("hb%d" % i) for i in range(NSUB)]
        SS = sb("SS", [128, 32]); bSS = [Buf("ss%d" % i) for i in range(8)]
        NEGH = sb("NEGH", [128, 1]); bNEGH = Buf("NEGH")
        HT = sb("HT", [128, 8, T], BF16); bHT = Buf("HT")
        SG = sb("SG", [128, 16, T], BF16); bSG = [Buf("sg%d" % i) for i in range(16)]
        YT = sb("YT", [128, 16, T], BF16); bYT = [Buf("yt%d" % i) for i in range(16)]
        LUr = [sb("lu%d" % i, [128, T + 3]) for i in range(2)]; bLU = [Buf("lu%d" % i) for i in range(2)]
        Lu = [sb("lu_%d" % i, [128, T]) for i in range(2)]; bLu = [Buf("lu_%d" % i) for i in range(2)]
        Lub = [sb("lub%d" % i, [128, T], BF16) for i in range(2)]; bLub = [Buf("lub%d" % i) for i in range(2)]
        Ltr = [sb("ltr%d" % i, [128, T]) for i in range(2)]; bLtr = [Buf("ltr%d" % i) for i in range(2)]
        Lti = [sb("lti%d" % i, [128, T]) for i in range(2)]; bLti = [Buf("lti%d" % i) for i in range(2)]
        Liu = [sb("liu%d" % i, [128, T], BF16) for i in range(4)]; bLiu = [Buf("liu%d" % i) for i in range(4)]
        La = [sb("la%d" % i, [128, T]) for i in range(4)]; bLa = [Buf("la%d" % i) for i in range(4)]
        Lm = [sb("lm%d" % i, [128, T]) for i in range(4)]; bLm = [Buf("lm%d" % i) for i in range(4)]
        Lh = [sb("lh%d" % i, [128, T]) for i in range(2)]; bLh = [Buf("lh%d" % i) for i in range(2)]
        LT = sb("LT", [128, T]); bLT = Buf("LT")
        QK0 = sb("qk0", [128, 512]); bQK0 = Buf("qk0")
        VT = [sb("vt%d" % i, [128, 512], BF16) for i in range(NSUB)]; bVT = [Buf("vt%d" % i) for i in range(NSUB)]
        GEj = [sb("GE%d" % i, [128, 256]) for i in range(NSUB)]; bGEj = [Buf("GE%d" % i) for i in range(NSUB)]
        GLAj = [sb("GLA%d" % i, [128, 256]) for i in range(NSUB)]; bGLAj = [Buf("GLA%d" % i) for i in range(NSUB)]
        GX = [sb("gx%d" % i, [128, 256]) for i in range(2)]; bGX = [Buf("gx%d" % i) for i in range(2)]
        QD = sb("QD", [128, 256], BF16); bQD = Buf("QD")
        KD = sb("KD", [128, 256], BF16); bKD = Buf("KD")
        KK = [sb("kk%d" % i, [128, 2, 256], BF16) for i in range(NSUB)]; bKK = [Buf("kk%d" % i) for i in range(NSUB)]
        QKT = sb("QKT", [128, 4, T], BF16); bQKT = Buf("QKT")
        DEC = sb("DEC", [128, 2, NCH]); bDEC = Buf("DEC")
        ATM = [sb("atm%d" % i, [128, 4, 128], BF16) for i in range(2)]; bATM = [Buf("atm%d" % i) for i in range(2)]
        GSQj = [sb("GSQ%d" % i, [128, 512], BF16) for i in range(NSUB)]; bGSQj = [Buf("GSQ%d" % i) for i in range(NSUB)]
        GRS = sb("GRS", [128, 512]); bGRS = Buf("GRS")
        QX = [sb("qx%d" % i, [128, 2, T], BF16) for i in range(1)]; bQX = [Buf("qx%d" % i) for i in range(1)]
        EX = [sb("ex%d" % i, [128, 2, T], BF16) for i in range(2)]; bEX = [Buf("ex%d" % i) for i in range(2)]
        RD = [sb("rd%d" % i, [128, T]) for i in range(2)]; bRD = [Buf("rd%d" % i) for i in range(2)]

        for i in range(NSUB):
            sch.op("pool", lambda e, i=i: e.memset(KK[i][:, :, :], 0.0), writes=[bKK[i]])
        sch.op("pool", lambda e: e.memset(NEGH[:, :], -0.5), writes=[bNEGH])

        pvc = lambda col: PV[:, col:col + 1]
        dpc = lambda col: DP[:, col:col + 1]
        ss_i = [0]

        def rms_rstd(src, bsrc, junk, bjunk, width):
            k = ss_i[0] % 8
            ss_i[0] += 1
            b = bSS[k]
            sch.op("act", lambda e: e.activation(out=junk, in_=src, func=AF.Square, accum_out=SS[:, 4 * k:4 * k + 1]), reads=[bsrc], writes=[bjunk, b])
            sch.op("pool", lambda e: e.tensor_scalar(out=SS[:, 4 * k + 1:4 * k + 2], in0=SS[:, 4 * k:4 * k + 1], scalar1=1.0 / width, scalar2=EPS, op0=ALU.mult, op1=ALU.add), reads=[b], writes=[b])
            sch.op("pool", lambda e: e.tensor_tensor(out=SS[:, 4 * k + 2:4 * k + 3], in0=SS[:, 4 * k + 1:4 * k + 2], in1=NEGH[:, 0:1], op=ALU.pow), reads=[b, bNEGH], writes=[b])
            return SS[:, 4 * k + 2:4 * k + 3], b

        def stageA_load(tt, j):
            t0 = tt * T + j * 128
            xi = xl_alloc()
            sch.dma("sp", lambda e, xi=xi, t0=t0: e.dma_start(out=XL[xi][:, :], in_=x_d[t0:t0 + 128, :]), "xl%d" % xi, writes=[bXL[xi]])
            rstd, brs = rms_rstd(XL[xi][:, :], bXL[xi], HB[j][:, :], bHB[j], D)
            sch.op("act", lambda e, xi=xi, j=j, rstd=rstd: e.activation(out=HB[j][:, :], in_=XL[xi][:, :], func=AF.Copy, scale=rstd), reads=[bXL[xi], brs], writes=[bHB[j]])

        def stageA_tr(j):
            for c in range(8):
                sch.op("pe", lambda e, c=c, j=j: e.transpose(out=PTB[:, c * 128:(c + 1) * 128], in_=HB[j][:, c * 128:(c + 1) * 128], identity=IDB[:, :]), reads=[bHB[j], bIDB], writes=[bPTB])
            sch.op("act", lambda e, j=j: e.activation(out=HT[:, :, j * 128:(j + 1) * 128], in_=PTB[:, :].rearrange("p (c t) -> p c t", c=8), func=AF.Copy), reads=[bPTB], writes=[bHT])

        def inproj_into(t, off, bp, col0, width=128):
            for dc in range(8):
                sch.op("pe", lambda e, dc=dc: e.matmul(out=t[0:width, off:off + T], lhsT=W1[:, dc, col0:col0 + width], rhs=HT[:, dc, :], start=(dc == 0), stop=(dc == 7)), reads=[bW1, bHT], writes=[bp])

        gate_done = set()
        inproj_left = [4]
        hb_cnt = [0]
        out_ok = [True]
        gate_i = [0]

        GATE_ORDER = (6, 0, 4, 7, 1, 5, 2, 3)
        NPRE = 4
        gate_pre = []

        def gate_mm(p):
            t, _, bp = pmm()
            inproj_into(t, 0, bp, O_GATE + 2 * p * 128)
            inproj_into(t, T, bp, O_GATE + (2 * p + 1) * 128)
            return t, bp

        def gate_preissue():
            for p in GATE_ORDER[:NPRE]:
                t, bp = gate_mm(p)
                gate_pre.append((p, t, bp))

        def gen_gate():
            pre = list(gate_pre)
            del gate_pre[:]
            todo = [(p, None, None) for p in GATE_ORDER[len(pre):]]
            first = True
            for (p, t, bp) in pre + todo:
                if t is None:
                    if pre and first:
                        first = False
                        yield
                    t, bp = gate_mm(p)
                    if p == GATE_ORDER[-1]:
                        inproj_left[0] -= 1
                sch.op("act", lambda e, t=t, p=p: e.activation(out=SG[:, 2 * p:2 * p + 2, :], in_=t[:, 0:2 * T].rearrange("p (c t) -> p c t", c=2), func=AF.Tanh, scale=0.5), reads=[bp], writes=bSG[2 * p:2 * p + 2])
                sch.op("dve", lambda e, t=t, p=p: e.scalar_tensor_tensor(out=SG[:, 2 * p:2 * p + 2, :], in0=SG[:, 2 * p:2 * p + 2, :], scalar=1.0, in1=t[:, 0:2 * T].rearrange("p (c t) -> p c t", c=2), op0=ALU.add, op1=ALU.mult), reads=[bp] + bSG[2 * p:2 * p + 2], writes=bSG[2 * p:2 * p + 2])
                gate_done.add(2 * p)
                gate_done.add(2 * p + 1)
                if t is not None and (p, t, bp) in pre:
                    continue
                yield
                if p not in GATE_ORDER[:3]:
                    yield

        def gla_tok_proj(j, col0, which):
            t, bp = ptok()
            for dc in range(8):
                sch.op("pe", lambda e, dc=dc: e.matmul(out=t[:, :], lhsT=HT[:, dc, j * 128:(j + 1) * 128], rhs=W1[:, dc, col0:col0 + 512], start=(dc == 0), stop=(dc == 7)), reads=[bW1, bHT], writes=[bp])
            if which == 0:
                sch.op("act", lambda e: e.activation(out=QK0[:, :], in_=t[:, :], func=AF.Copy), reads=[bp], writes=[bQK0])
            else:
                sch.op("act", lambda e: e.activation(out=VT[j][:, :], in_=t[:, :], func=AF.Copy), reads=[bp], writes=[bVT[j]])

        def gen_gla():
            t, _, bp = pmm()
            inproj_into(t, 0, bp, O_GLR, 16)
            sch.op("act", lambda e, t=t: e.activation(out=GLR[0:16, :], in_=t[0:16, 0:T], func=AF.Copy), reads=[bp], writes=[bGLR])
            yield
            for j in range(NSUB):
                gla_tok_proj(j, O_V, 1)
                yield
            for j in range(NSUB):
                t, _, bp = pmm()
                sch.op("pe", lambda e, t=t, j=j: e.matmul(out=t[:, 0:256], lhsT=GLR[:, j * 128:(j + 1) * 128], rhs=WG[:, :], start=True, stop=True), reads=[bGLR, bWG], writes=[bp])
                sch.op("act", lambda e, t=t, j=j: e.activation(out=GEj[j][:, :], in_=t[:, 0:256], func=AF.Exp, scale=-1.0), reads=[bp], writes=[bGEj[j]])
            for j in range(NSUB):
                sch.op("act", lambda e, j=j: e.activation(out=GLAj[j][:, :], in_=GEj[j][:, :], func=AF.Ln, bias=1.0), reads=[bGEj[j]], writes=[bGLAj[j]])
            yield
            for j in range(NSUB):
                gla_tok_proj(j, O_Q, 0)
                if j == NSUB - 1:
                    inproj_left[0] -= 1
                yield
                LA, bLA, EK, bEK = GLAj[j], bGLAj[j], GEj[j], bGEj[j]
                tc_, _, bpc = pmm()
                sch.op("pe", lambda e, t=tc_, LA=LA: e.matmul(out=t[:, 0:256], lhsT=CST[:, C_L:C_L + 128], rhs=LA[:, :], start=True, stop=True), reads=[bCST, bLA], writes=[bpc])
                sch.op("pe", lambda e, t=tc_, LA=LA: e.matmul(out=t[:, 256:512], lhsT=CST[:, C_U:C_U + 128], rhs=LA[:, :], start=True, stop=True), reads=[bCST, bLA], writes=[bpc])
                td_, _, bpd = pmm()
                for kc in range(2):
                    sch.op("pe", lambda e, t=td_, kc=kc, LA=LA: e.matmul(out=t[:, kc * 2:kc * 2 + 2], lhsT=LA[:, kc * 128:(kc + 1) * 128], rhs=CST[:, C_SEL:C_SEL + 2], start=True, stop=True), reads=[bCST, bLA], writes=[bpd])
                sch.op("act", lambda e, t=tc_: e.activation(out=GX[0][:, :], in_=t[:, 0:256], func=AF.Exp, scale=-1.0 / 16), reads=[bpc], writes=[bGX[0]])
                sch.op("act", lambda e, t=tc_: e.activation(out=GX[1][:, :], in_=t[:, 0:256], func=AF.Exp, scale=1.0 / 16), reads=[bpc], writes=[bGX[1]])
                sch.op("act", lambda e, t=tc_, EK=EK: e.activation(out=EK[:, :], in_=t[:, 256:512], func=AF.Exp, scale=-1.0 / 16), reads=[bpc], writes=[bEK])
                sch.op("act", lambda e, t=td_, j=j: e.activation(out=DEC[:, :, 2 * j:2 * j + 2], in_=t[:, 0:4].rearrange("p (k n) -> p k n", k=2), func=AF.Exp, scale=-1.0 / 16), reads=[bpd], writes=[bDEC])
                sch.op("dve", lambda e: e.scalar_tensor_tensor(out=QD[:, :], in0=QK0[:, 0:256], scalar=0.125, in1=GX[0][:, :], op0=ALU.mult, op1=ALU.mult), reads=[bQK0, bGX[0]], writes=[bQD])
                sch.op("dve", lambda e: e.tensor_tensor(out=KD[:, :], in0=QK0[:, 256:512], in1=GX[1][:, :], op=ALU.mult), reads=[bQK0, bGX[1]], writes=[bKD])
                for hf in range(2):
                    sch.op("dve", lambda e, j=j, hf=hf, EK=EK: e.tensor_tensor(out=KK[j][hf * 64:(hf + 1) * 64, hf, :], in0=QK0[hf * 64:(hf + 1) * 64, 256:512], in1=EK[hf * 64:(hf + 1) * 64, :], op=ALU.mult), reads=[bQK0, bEK], writes=[bKK[j]])
                for i4, (src, bsrc) in enumerate(((QD, bQD), (QD, bQD), (KD, bKD), (KD, bKD))):
                    kc = i4 % 2
                    sch.op("pe", lambda e, i4=i4, kc=kc, src=src: e.transpose(out=PTB[:, i4 * 128:(i4 + 1) * 128], in_=src[:, kc * 128:(kc + 1) * 128], identity=IDB[:, :]), reads=[bsrc, bIDB], writes=[bPTB])
                sch.op("act", lambda e, j=j: e.activation(out=QKT[:, :, j * 128:(j + 1) * 128], in_=PTB[:, 0:512].rearrange("p (c t) -> p c t", c=4), func=AF.Copy), reads=[bPTB], writes=[bQKT])
                yield
            for j in range(NSUB):
                tsl = slice(j * 128, (j + 1) * 128)
                am = j % 2
                te, _, bte = pmm()
                to, _, bto = pmm()
                for h in range(4):
                    kc, hh = h // 2, h % 2
                    tgt, bt = (te, bte) if hh == 0 else (to, bto)
                    sch.op("pe", lambda e, kc=kc, hh=hh, tgt=tgt: e.matmul(out=tgt[:, kc * 128:(kc + 1) * 128], lhsT=QKT[hh * 64:(hh + 1) * 64, 2 + kc, tsl], rhs=QKT[hh * 64:(hh + 1) * 64, kc, tsl], start=True, stop=True), reads=[bQKT], writes=[bt])
                for hh, (tgt, bt) in enumerate(((te, bte), (to, bto))):
                    for kc in range(2):
                        h = 2 * kc + hh
                        sch.op("dve", lambda e, tgt=tgt, kc=kc, h=h, am=am: e.tensor_tensor(out=ATM[am][:, h, :], in0=tgt[:, kc * 128:(kc + 1) * 128], in1=CST[:, C_L:C_L + 128], op=ALU.mult), reads=[bt, bCST], writes=[bATM[am]])
                yield
                for hf in range(2):
                    n = 2 * j + hf
                    csl = slice(j * 128 + hf * 64, j * 128 + hf * 64 + 64)
                    for h in range(4):
                        kc = h // 2
                        sch.op("pe", lambda e, h=h, hf=hf: e.matmul(out=PB[6][:, h * 128 + hf * 64:h * 128 + hf * 64 + 64], lhsT=VT[j][:, h * 128:(h + 1) * 128], rhs=ATM[am][:, h, hf * 64:(hf + 1) * 64], start=True, stop=False), reads=[bVT[j], bATM[am]], writes=[bOPS])
                        sch.op("pe", lambda e, h=h, hf=hf, kc=kc, csl=csl: e.matmul(out=PB[6][:, h * 128 + hf * 64:h * 128 + hf * 64 + 64], lhsT=SBF[:, h, :], rhs=QKT[:, kc, csl], start=False, stop=True), reads=[bSBF, bQKT], writes=[bOPS])
                    tk, _, bpk = pmm()
                    for kc in range(2):
                        sch.op("pe", lambda e, kc=kc, hf=hf, tk=tk: e.matmul(out=tk[:, kc * 256:(kc + 1) * 256], lhsT=KK[j][:, hf, kc * 128:(kc + 1) * 128], rhs=VT[j][:, kc * 256:(kc + 1) * 256], start=True, stop=True), reads=[bKK[j], bVT[j]], writes=[bpk])
                    for kc in range(2):
                        for hh in range(2):
                            psl = slice(hh * 64, (hh + 1) * 64)
                            sch.op("dve", lambda e, kc=kc, hh=hh, psl=psl, n=n, tk=tk: e.scalar_tensor_tensor(out=S32[psl, kc, :], in0=S32[psl, kc, :], scalar=DEC[psl, kc, n:n + 1], in1=tk[psl, kc * 256 + hh * 128:kc * 256 + (hh + 1) * 128], op0=ALU.mult, op1=ALU.add), reads=[bS32, bDEC, bpk], writes=[bS32])
                    for hh in range(2):
                        psl = slice(hh * 64, (hh + 1) * 64)
                        sch.op("dve", lambda e, hh=hh, psl=psl: e.tensor_copy(out=SBF[psl, hh:4:2, :], in_=S32[psl, 0:2, :]), reads=[bS32], writes=[bSBF])
                    yield
                    yield
                sch.op("act", lambda e, j=j: e.activation(out=GSQj[j][:, :], in_=PB[6][:, :], func=AF.Square), reads=[bOPS], writes=[bGSQj[j]])
                while not (out_ok[0] and all((8 + h) in gate_done for h in range(4))):
                    yield
                for h in range(4):
                    fc = 8 + h
                    sch.op("dve", lambda e, h=h, fc=fc: e.scalar_tensor_tensor(out=YT[:, fc, tsl], in0=PB[6][:, h * 128:(h + 1) * 128], scalar=pvc(PV_GNG + h), in1=SG[:, fc, tsl], op0=ALU.mult, op1=ALU.mult), reads=[bOPS, bPV, bSG[fc]], writes=[bYT[fc]])
                yield
            hb_ = [ptok() for _ in range(NSUB)]
            for j in range(NSUB):
                t, bp = hb_[j]
                sch.op("pe", lambda e, t=t, j=j: e.matmul(out=t[:, :], lhsT=ONB[:, :], rhs=GSQj[j][:, :], start=True, stop=True), reads=[bONB, bGSQj[j]], writes=[bp])
            for j in range(NSUB):
                t, bp = hb_[j]
                sch.op("act", lambda e, t=t: e.activation(out=t[:, :], in_=t[:, :], func=AF.Ln, scale=1.0 / 128, bias=EPS), reads=[bp], writes=[bp])
            for j in range(NSUB):
                t, bp = hb_[j]
                tsl = slice(j * 128, (j + 1) * 128)
                sch.op("act", lambda e, t=t: e.activation(out=GRS[:, :], in_=t[:, :], func=AF.Exp, scale=-0.5), reads=[bp], writes=[bGRS])
                sch.op("dve", lambda e, tsl=tsl: e.scalar_tensor_tensor(out=YT[:, 8:12, tsl], in0=YT[:, 8:12, tsl], scalar=0.5, in1=GRS[:, :].rearrange("p (h t) -> p h t", h=4), op0=ALU.mult, op1=ALU.mult), reads=[bGRS] + bYT[8:12], writes=bYT[8:12])
            yield

        def gen_xa():
            for hp in range(2):
                t, _, bp = pmm()
                for q in range(2):
                    inproj_into(t, q * T, bp, O_XQ + (2 * hp + q) * 128)
                if hp == 1:
                    inproj_left[0] -= 1
                sch.op("act", lambda e, t=t: e.activation(out=QX[0][:, :, :], in_=t[:, 0:2 * T].rearrange("p (c t) -> p c t", c=2), func=AF.Copy), reads=[bp], writes=[bQX[0]])
                yield
                for q in range(2):
                    h = 2 * hp + q
                    k = h % 2
                    fc = 12 + h
                    ts_, _, bps = pmm()
                    for mc in range(2):
                        sch.op("pe", lambda e, mc=mc, h=h, q=q, ts_=ts_: e.matmul(out=ts_[:, mc * T:(mc + 1) * T], lhsT=KT[:, h, mc * 128:(mc + 1) * 128], rhs=QX[0][:, q, :], start=True, stop=True), reads=[bKT, bQX[0]], writes=[bps])
                    sch.op("act", lambda e, k=k, ts_=ts_: e.activation(out=EX[k][:, :, :], in_=ts_[:, 0:2 * T].rearrange("p (c t) -> p c t", c=2), func=AF.Exp, scale=128.0 ** -0.5), reads=[bps], writes=[bEX[k]])
                    yield
                    to_, _, bpo = pmm()
                    for mc in range(2):
                        sch.op("pe", lambda e, mc=mc, h=h, k=k, to_=to_: e.matmul(out=to_[:, 0:T], lhsT=VM[:, mc, h * 128:(h + 1) * 128], rhs=EX[k][:, mc, :], start=(mc == 0), stop=(mc == 1)), reads=[bVM, bEX[k]], writes=[bpo])
                    for mc in range(2):
                        sch.op("pe", lambda e, mc=mc, k=k, to_=to_: e.matmul(out=to_[:, T:2 * T], lhsT=ONB[:, :], rhs=EX[k][:, mc, :], start=(mc == 0), stop=(mc == 1)), reads=[bONB, bEX[k]], writes=[bpo])
                    sch.op("dve", lambda e, k=k, to_=to_: e.reciprocal(out=RD[k][:, :], in_=to_[:, T:2 * T]), reads=[bpo], writes=[bRD[k]])
                    sch.op("dve", lambda e, k=k, to_=to_: e.scalar_tensor_tensor(out=RD[k][:, :], in0=to_[:, 0:T], scalar=0.5, in1=RD[k][:, :], op0=ALU.mult, op1=ALU.mult), reads=[bpo, bRD[k]], writes=[bRD[k]])
                    while not (out_ok[0] and fc in gate_done):
                        yield
                    sch.op("pool", lambda e, k=k, fc=fc: e.tensor_tensor(out=YT[:, fc, :], in0=RD[k][:, :], in1=SG[:, fc, :], op=ALU.mult), reads=[bRD[k], bSG[fc]], writes=[bYT[fc]])
                    yield

        lru_i = [0]

        def gen_lru():
            def stepA(c):
                s = c % 2
                t, _, bp = pmm()
                inproj_into(t, 0, bp, O_LRU + c * 128)
                if c == 7:
                    inproj_left[0] -= 1
                sch.op("dve", lambda e: e.tensor_copy(out=LUr[s][:, 3:3 + T], in_=t[:, 0:T]), reads=[bp], writes=[bLU[s]])
                sch.op("pool", lambda e: e.tensor_copy(out=LUr[s][:, 0:3], in_=UH[:, c, :]), reads=[bUH], writes=[bLU[s]])
                cw = lambda k_: pvc(PV_CONVW + c * 4 + k_)
                sch.op("pool", lambda e: e.tensor_scalar(out=Lu[s][:, :], in0=LUr[s][:, 3:3 + T], scalar1=cw(3), scalar2=pvc(PV_CONVB + c), op0=ALU.mult, op1=ALU.add), reads=[bLU[s], bPV], writes=[bLu[s]])
                sch.op("pool", lambda e: e.tensor_scalar(out=LT[:, :], in0=LUr[s][:, 2:2 + T], scalar1=cw(2), scalar2=0.0, op0=ALU.mult, op1=ALU.add), reads=[bLU[s], bPV], writes=[bLT])
                sch.op("pool", lambda e: e.tensor_tensor(out=Lu[s][:, :], in0=Lu[s][:, :], in1=LT[:, :], op=ALU.add), reads=[bLT, bLu[s]], writes=[bLu[s]])
                for k_ in (1, 0):
                    sch.op("pool", lambda e: e.tensor_scalar(out=LT[:, :], in0=LUr[s][:, k_:k_ + T], scalar1=cw(k_), scalar2=0.0, op0=ALU.mult, op1=ALU.add), reads=[bLU[s], bPV], writes=[bLT])
                    sch.op("pool", lambda e: e.tensor_tensor(out=Lu[s][:, :], in0=Lu[s][:, :], in1=LT[:, :], op=ALU.add), reads=[bLT, bLu[s]], writes=[bLu[s]])
                sch.op("pool", lambda e: e.tensor_copy(out=UH[:, c, :], in_=LUr[s][:, T:T + 3]), reads=[bLU[s]], writes=[bUH])
                sch.op("pool", lambda e: e.tensor_copy(out=Lub[s][:, :], in_=Lu[s][:, :]), reads=[bLu[s]], writes=[bLub[s]])

            def stepB(c):
                s = c % 2
                r4 = c % 4
                tz, _, bpz = pmm()
                sch.op("pe", lambda e: e.matmul(out=tz[:, 0:T], lhsT=WA[:, c, :], rhs=Lub[s][:, :], start=True, stop=True), reads=[bWA, bLub[s]], writes=[bpz])
                sch.op("pe", lambda e: e.matmul(out=tz[:, T:2 * T], lhsT=WI[:, c, :], rhs=Lub[s][:, :], start=True, stop=True), reads=[bWI, bLub[s]], writes=[bpz])
                sch.op("act", lambda e: e.activation(out=Ltr[s][:, :], in_=tz[:, 0:T], func=AF.Tanh, scale=0.5, bias=dpc(c)), reads=[bpz, bDP], writes=[bLtr[s]])
                sch.op("act", lambda e: e.activation(out=Lti[s][:, :], in_=tz[:, T:2 * T], func=AF.Tanh, scale=0.5, bias=dpc(8 + c)), reads=[bpz, bDP], writes=[bLti[s]])
                sch.op("act", lambda e: e.activation(out=La[r4][:, :], in_=Ltr[s][:, :], func=AF.Exp, scale=dpc(16 + c), bias=dpc(16 + c)), reads=[bLtr[s], bDP], writes=[bLa[r4]])
                sch.op("act", lambda e: e.activation(out=Lm[r4][:, :], in_=Ltr[s][:, :], func=AF.Exp, scale=dpc(24 + c), bias=dpc(24 + c)), reads=[bLtr[s], bDP], writes=[bLm[r4]])
                sch.op("dve", lambda e: e.scalar_tensor_tensor(out=Liu[r4][:, :], in0=Lti[s][:, :], scalar=1.0, in1=Lu[s][:, :], op0=ALU.add, op1=ALU.mult), reads=[bLti[s], bLu[s]], writes=[bLiu[r4]])

            def stepL():
                for r4 in range(4):
                    sch.op("act", lambda e: e.activation(out=Lm[r4][:, :], in_=Lm[r4][:, :], func=AF.Sqrt, scale=-0.0625, bias=0.0625), reads=[bLm[r4]], writes=[bLm[r4]])

            def stepC(c):
                r4 = c % 4
                s2 = c % 2
                sch.op("dve", lambda e: e.tensor_tensor(out=Lm[r4][:, :], in0=Lm[r4][:, :], in1=Liu[r4][:, :], op=ALU.mult), reads=[bLm[r4], bLiu[r4]], writes=[bLm[r4]])
                sch.op("dve", lambda e: e.tensor_tensor_scan(out=Lh[s2][:, :], data0=La[r4][:, :], data1=Lm[r4][:, :], initial=HST[:, c:c + 1], op0=ALU.mult, op1=ALU.add), reads=[bLa[r4], bLm[r4], bHST], writes=[bLh[s2]])
                sch.op("pool", lambda e: e.tensor_copy(out=HST[:, c:c + 1], in_=Lh[s2][:, T - 1:T]), reads=[bLh[s2]], writes=[bHST])
                while not (out_ok[0] and c in gate_done):
                    yield
                sch.op("pool", lambda e: e.tensor_tensor(out=YT[:, c, :], in0=Lh[s2][:, :], in1=SG[:, c, :], op=ALU.mult), reads=[bLh[s2], bSG[c]], writes=[bYT[c]])

            order = ["A0", "A1", "B0", "A2", "B1", "A3", "B2", "A4", "B3", "A5", "L", "C0", "B4", "C1", "A6", "B5", "C2", "A7", "B6", "C3", "B7", "L", "C4", "C5", "C6", "C7"]
            for st in order:
                if st[0] == "A":
                    stepA(int(st[1]))
                elif st[0] == "B":
                    stepB(int(st[1]))
                elif st[0] == "L":
                    stepL()
                else:
                    yield from stepC(int(st[1]))
                yield

        def gen_prefetch(tt):
            for _ in range(6):
                yield
            while not out_ok[0]:
                yield
            if tt + 1 < NT:
                for j in range(NSUB):
                    stageA_load(tt + 1, j)
                    hb_cnt[0] += 1
                    yield
            for j in range(NSUB):
                t0 = tt * T + j * 128
                xi = xl_alloc()
                sch.dma("sp", lambda e, xi=xi, t0=t0: e.dma_start(out=XL[xi][:, :], in_=x_d[t0:t0 + 128, :]), "xl%d" % xi, writes=[bXL[xi]])
                xo_slots.append(xi)
                yield

        def gen_tr(tt):
            if tt + 1 < NT:
                while inproj_left[0] > 0 or hb_cnt[0] < NSUB:
                    yield
                for j in range(NSUB):
                    stageA_tr(j)
                yield

        xo_slots = []

        def out_stage(tt):
            for j in range(NSUB):
                t0 = tt * T + j * 128
                xi = xo_slots.pop(0)
                for nh in range(2):
                    t, bp = ptok()
                    for fc in range(16):
                        sch.op("pe", lambda e, t=t, fc=fc, j=j, nh=nh: e.matmul(out=t[:, :], lhsT=YT[:, fc, j * 128:(j + 1) * 128], rhs=W2[:, fc, nh * 512:(nh + 1) * 512], start=(fc == 0), stop=(fc == 15)), reads=[bYT[fc], bW2], writes=[bp])
                    sch.op("dve", lambda e, t=t, xi=xi, nh=nh: e.tensor_tensor(out=XL[xi][:, nh * 512:(nh + 1) * 512], in0=XL[xi][:, nh * 512:(nh + 1) * 512], in1=t[:, :], op=ALU.add), reads=[bXL[xi], bp], writes=[bXL[xi]])
                rstd, brs = rms_rstd(XL[xi][:, :], bXL[xi], HB[j][:, :], bHB[j], D)
                sch.op("dve", lambda e, xi=xi, rstd=rstd: e.scalar_tensor_tensor(out=XL[xi][:, :], in0=XL[xi][:, :], scalar=rstd, in1=FGB[:, :], op0=ALU.mult, op1=ALU.mult), reads=[bXL[xi], brs, bFGB], writes=[bXL[xi]])
                sch.dma("sp", lambda e, xi=xi, t0=t0: e.dma_start(out=out_d[t0:t0 + 128, :], in_=XL[xi][:, :]), "xo%d" % xi, reads=[bXL[xi]])


        def interleave(gens, rounds=None):
            while gens and (rounds is None or rounds > 0):
                if rounds is not None:
                    rounds -= 1
                for gw in list(gens):
                    g, w = gw
                    for _ in range(w):
                        try:
                            next(g)
                        except StopIteration:
                            gens.remove(gw)
                            break

        for j in range(NSUB):
            stageA_load(0, j)
        EARLY_ROUNDS = 2
        for j in range(NSUB):
            stageA_tr(j)
        def mk_streams(tt_):
            inproj_left[0] = 4
            hb_cnt[0] = 0
            return [[gen_gate(), 2], [gen_gla(), 2], [gen_lru(), 2], [gen_xa(), 1], [gen_prefetch(tt_), 1], [gen_tr(tt_), 1]]
        streams = mk_streams(0)
        for tt in range(NT):
            interleave(streams)
            gate_done.clear()
            if tt + 1 < NT:
                out_ok[0] = False
                streams = mk_streams(tt + 1)
                interleave(streams, rounds=EARLY_ROUNDS)
            out_stage(tt)
            out_ok[0] = True

        sch.finish("sp")

        for key in list(sch.dcnt.keys()):
            get_sem(key)

        def replay(name, eng):
            for cmd in sch.cmds[name]:
                if cmd[0] == "wait":
                    eng.wait_ge(sems[cmd[1]], cmd[2])
                else:
                    _, fn, key, inc = cmd
                    fn(eng).then_inc(sems[key], inc)

        with nc.Block() as block:
            @block.sync
            def _(e):
                replay("sp", e)

            @block.scalar
            def _(e):
                replay("act", e)

            @block.gpsimd
            def _(e):
                replay("pool", e)

            @block.tensor
            def _(e):
                replay("pe", e)

            @block.vector
            def _(e):
                replay("dve", e)
    return nc


def _consts():
    c = np.zeros((128, NCC), np.float32)
    idx = np.arange(128)
    c[:, C_ID:C_ID + 128] = np.eye(128, dtype=np.float32)
    same = (idx[:, None] // 64) == (idx[None, :] // 64)
    c[:, C_L:C_L + 128] = (same & (idx[:, None] <= idx[None, :])).astype(np.float32)
    c[:, C_U:C_U + 128] = (same & (idx[:, None] > idx[None, :])).astype(np.float32)
    c[:, C_SEL] = (idx < 64).astype(np.float32)
    c[:, C_SEL + 1] = (idx >= 64).astype(np.float32)
    c[:, C_ONE:C_ONE + 128] = 1.0
    return c


def host_layout(inp, b, S):
    f = lambda a: np.ascontiguousarray(np.asarray(a, dtype=np.float32))
    chunked = lambda v: f(v).reshape(-1, 128).T
    pv = np.zeros((128, NPV), np.float32)
    cw = f(inp["conv_w"][0])
    for c in range(8):
        for k in range(4):
            pv[:, PV_CONVW + c * 4 + k] = cw[k, c * 128:(c + 1) * 128]
    pv[:, PV_CONVB:PV_CONVB + 8] = chunked(inp["conv_b"][0])
    pv[:, PV_BA:PV_BA + 8] = f(inp["lru_b_a"][0]).T
    pv[:, PV_BI:PV_BI + 8] = f(inp["lru_b_i"][0]).T
    pv[:, PV_LAM:PV_LAM + 8] = chunked(inp["lru_lambda"][0])
    pv[:, PV_NG:PV_NG + 8] = chunked(inp["norm_g"][0])
    pv[:, PV_MNG:PV_MNG + 8] = chunked(inp["mem_norm_g"][0])
    pv[:, PV_GNG:PV_GNG + 4] = chunked(inp["gla_norm_g"][0])
    wg2b = np.concatenate([f(inp["gla_w_g2"][0]), f(inp["gla_b_g"][0])[None, :]], axis=0)
    return {
        "x": f(inp["x"][b, :S]),
        "mem": f(inp["mem"][b]),
        "w_in": f(inp["w_in"][0]),
        "w_mem_kv": f(inp["w_mem_kv"][0]),
        "w_out": f(inp["w_out"][0]),
        "wa": f(np.transpose(f(inp["lru_w_a"][0]), (1, 0, 2)).reshape(128, 1024)),
        "wi": f(np.transpose(f(inp["lru_w_i"][0]), (1, 0, 2)).reshape(128, 1024)),
        "pvec": pv,
        "wg2b": f(wg2b),
        "fgb": f(np.broadcast_to(f(inp["final_norm_g"])[None, :], (128, D))),
        "cst": _consts(),
    }


def kernel(**inputs):
    nc = build(SEQ)
    in_maps = [host_layout(inputs, b, SEQ) for b in range(NB)]
    res = run_bass_kernel_spmd(nc, in_maps, core_ids=list(range(NB)))
    return np.stack([np.asarray(r["out"], dtype=np.float32) for r in res.results], axis=0)
```
